# Optimizing a Trainium2 kernel written in Bass

```python
import math
import jax, jax.numpy as jnp
from jax import lax
import numpy as np

D_MODEL = 1024
BATCH = 16
SEQ = 2048
DEPTH = 2

CHUNK = 64
N_META = 16
BLOCK = math.gcd(N_META, CHUNK)
N_MIXERS = 2
N_LAYERS_A = (DEPTH + N_MIXERS - 1) // N_MIXERS
N_LAYERS_B = DEPTH // N_MIXERS
EPS = 1e-6

D_FF = 2816

A_HEADS = 8
A_DK = D_MODEL // 2 // A_HEADS
A_DV = D_MODEL // A_HEADS
A_CONV = 4
A_QK = A_HEADS * A_DK
A_V = A_HEADS * A_DV
A_COLS = 2 * A_QK + 2 * A_V + 2 * A_HEADS

B_HEADS = 8
B_DK = 128
B_DV = D_MODEL // B_HEADS
B_F = B_HEADS * B_DK
B_I = B_HEADS * B_DV
B_COLS = 2 * B_F + 2 * B_I

kernel_name = 'hybrid_mlstm_hgrn2_macaron_trunk'


def rmsnorm(x, g):
    xf = x.astype(jnp.float32)
    y = xf * lax.rsqrt(jnp.mean(xf * xf, axis=-1, keepdims=True) + EPS)
    return (y * g.astype(jnp.float32)).astype(x.dtype)


def swiglu(x, w_in, w_out):
    gu = x @ w_in
    return (jax.nn.silu(gu[..., :D_FF]) * gu[..., D_FF:]) @ w_out


def causal_conv(x, w, b):
    kw, c = w.shape
    y = lax.conv_general_dilated(x, w[:, None, :].astype(x.dtype), window_strides=(1,),
                                 padding=[(kw - 1, 0)], dimension_numbers=('NWC', 'WIO', 'NWC'),
                                 feature_group_count=c)
    return y + b.astype(y.dtype)


def to_blocks(x):
    bsz, t = x.shape[0], x.shape[1]
    x = x.reshape((bsz, t // BLOCK, BLOCK) + x.shape[2:])
    return jnp.swapaxes(jnp.moveaxis(x, 1, 0), 2, 3)


def from_blocks(x):
    nb, bsz, h, l, d = x.shape
    x = jnp.moveaxis(jnp.swapaxes(x, 2, 3), 0, 1)
    return x.reshape(bsz, nb * l, h * d)


def mlstm_scan(q, k, v, ig, lf):
    _, bsz, nh, l, dk = q.shape
    dv = v.shape[-1]
    causal = jnp.tril(jnp.ones((l, l), dtype=bool))

    def step(carry, xs):
        c_prev, n_prev, m_prev = carry
        qb, kb, vb, ib, fb = xs
        bcum = jnp.cumsum(fb, axis=-1)
        dmat = jnp.where(causal, bcum[..., :, None] - bcum[..., None, :] + ib[..., None, :], -jnp.inf)
        g_inter = bcum + m_prev[..., None]
        m = jnp.maximum(g_inter, jnp.max(dmat, axis=-1))
        w_intra = jnp.exp(dmat - m[..., None])
        w_inter = jnp.exp(g_inter - m)
        s = jnp.einsum('bhld,bhsd->bhls', qb, kb) * w_intra
        num = jnp.einsum('bhls,bhsv->bhlv', s, vb) + w_inter[..., None] * jnp.einsum('bhvd,bhld->bhlv', c_prev, qb)
        den = jnp.sum(s, axis=-1) + w_inter * jnp.einsum('bhd,bhld->bhl', n_prev, qb)
        hb = num / jnp.maximum(jnp.abs(den), jnp.exp(-m))[..., None]
        b_last = bcum[..., -1]
        g_state = b_last[..., None] - bcum + ib
        m_new = jnp.maximum(b_last + m_prev, jnp.max(g_state, axis=-1))
        w_s = jnp.exp(g_state - m_new[..., None])
        decay = jnp.exp(b_last + m_prev - m_new)
        c_new = decay[..., None, None] * c_prev + jnp.einsum('bhsv,bhsd->bhvd', vb * w_s[..., None], kb)
        n_new = decay[..., None] * n_prev + jnp.einsum('bhs,bhsd->bhd', w_s, kb)
        return (c_new, n_new, m_new), hb

    init = (jnp.zeros((bsz, nh, dv, dk), jnp.float32), jnp.zeros((bsz, nh, dk), jnp.float32),
            jnp.zeros((bsz, nh), jnp.float32))
    _, hs = lax.scan(step, init, (q, k, v, ig, lf))
    return hs


def hgrn2_scan(q, k, v, lf):
    _, bsz, nh, l, dk = q.shape
    dv = v.shape[-1]
    causal = jnp.tril(jnp.ones((l, l), dtype=bool))

    def step(s_prev, xs):
        qb, kb, vb, fb = xs
        bcum = jnp.cumsum(fb, axis=-2)
        rel = jnp.where(causal[:, :, None], bcum[..., :, None, :] - bcum[..., None, :, :], -jnp.inf)
        a = jnp.sum(qb[..., :, None, :] * jnp.exp(rel) * kb[..., None, :, :], axis=-1)
        o = jnp.einsum('bhls,bhsv->bhlv', a, vb) + jnp.einsum('bhld,bhdv->bhlv', qb * jnp.exp(bcum), s_prev)
        b_last = bcum[..., -1, :]
        s_new = jnp.exp(b_last)[..., None] * s_prev + jnp.einsum(
            'bhsd,bhsv->bhdv', kb * jnp.exp(b_last[..., None, :] - bcum), vb)
        return s_new, o

    _, os_ = lax.scan(step, jnp.zeros((bsz, nh, dk, dv), jnp.float32), (q, k, v, lf))
    return os_


def mlstm_mixer(u, w_in, conv_w, conv_b, gate_b, w_out):
    bsz, t, _ = u.shape
    z = u @ w_in
    qk = jax.nn.silu(causal_conv(z[..., :2 * A_QK], conv_w, conv_b))
    v = z[..., 2 * A_QK:2 * A_QK + A_V]
    og = z[..., 2 * A_QK + A_V:2 * A_QK + 2 * A_V]
    gates = z[..., 2 * A_QK + 2 * A_V:].astype(jnp.float32) + gate_b.astype(jnp.float32)
    q = qk[..., :A_QK].reshape(bsz, t, A_HEADS, A_DK).astype(jnp.float32) * (A_DK ** -0.5)
    k = qk[..., A_QK:].reshape(bsz, t, A_HEADS, A_DK).astype(jnp.float32)
    v = v.reshape(bsz, t, A_HEADS, A_DV).astype(jnp.float32)
    ig = gates[..., :A_HEADS]
    lf = jax.nn.log_sigmoid(gates[..., A_HEADS:])
    hs = mlstm_scan(to_blocks(q), to_blocks(k), to_blocks(v), to_blocks(ig), to_blocks(lf))
    y = from_blocks(hs).astype(u.dtype) * jax.nn.sigmoid(og)
    return y @ w_out


def hgrn2_mixer(u, w_in, f_bias, lower_bound, g_norm, w_out):
    bsz, t, _ = u.shape
    z = u @ w_in
    q = jax.nn.silu(z[..., :B_F])
    fpre = z[..., B_F:2 * B_F].astype(jnp.float32) + f_bias.astype(jnp.float32)
    i_in = z[..., 2 * B_F:2 * B_F + B_I]
    g = z[..., 2 * B_F + B_I:]
    lf = jnp.logaddexp(jnp.log(lower_bound), jnp.log1p(-lower_bound) + jax.nn.log_sigmoid(fpre))
    k = (1.0 - lower_bound) * jax.nn.sigmoid(-fpre)
    q = q.reshape(bsz, t, B_HEADS, B_DK).astype(jnp.float32)
    k = k.reshape(bsz, t, B_HEADS, B_DK)
    lf = lf.reshape(bsz, t, B_HEADS, B_DK)
    v = i_in.reshape(bsz, t, B_HEADS, B_DV).astype(jnp.float32)
    o = hgrn2_scan(to_blocks(q), to_blocks(k), to_blocks(v), to_blocks(lf))
    y = from_blocks(o).astype(u.dtype) * jax.nn.sigmoid(g)
    return rmsnorm(y, g_norm) @ w_out


def setup_inputs(seed: int = 0) -> dict:
    key = jax.random.key(seed)
    ks = jax.random.split(key, 16)
    f32 = jnp.float32
    x = jax.random.normal(ks[0], (BATCH, SEQ, D_MODEL), f32)
    meta_tokens = jax.random.normal(ks[1], (N_META, D_MODEL), f32)
    norm_gains = 1.0 + 0.1 * jax.random.normal(ks[2], (DEPTH, 6, D_MODEL), f32)
    ffn_w_in = jax.random.normal(ks[3], (DEPTH, 2, D_MODEL, 2 * D_FF), f32) * D_MODEL ** -0.5
    ffn_w_out = jax.random.normal(ks[4], (DEPTH, 2, D_FF, D_MODEL), f32) * D_FF ** -0.5
    a_w_in = jax.random.normal(ks[5], (N_LAYERS_A, D_MODEL, A_COLS), f32) * D_MODEL ** -0.5
    a_conv_w = jax.random.normal(ks[6], (N_LAYERS_A, A_CONV, 2 * A_QK), f32) * A_CONV ** -0.5
    a_conv_b = 0.01 * jax.random.normal(ks[7], (N_LAYERS_A, 2 * A_QK), f32)
    i_bias = 0.1 * jax.random.normal(ks[8], (N_LAYERS_A, A_HEADS), f32)
    f_bias_a = jnp.linspace(3.0, 6.0, A_HEADS, dtype=f32)[None, :] + 0.1 * jax.random.normal(ks[9], (N_LAYERS_A, A_HEADS), f32)
    a_gate_b = jnp.concatenate([i_bias, f_bias_a], axis=-1)
    a_w_out = jax.random.normal(ks[10], (N_LAYERS_A, A_V, D_MODEL), f32) * A_V ** -0.5
    b_w_in = jax.random.normal(ks[11], (N_LAYERS_B, D_MODEL, B_COLS), f32) * D_MODEL ** -0.5
    b_f_bias = 0.1 * jax.random.normal(ks[12], (N_LAYERS_B, B_F), f32)
    b_lb_raw = 0.5 * jax.random.normal(ks[13], (DEPTH, B_F), f32)
    b_g_norm = 1.0 + 0.1 * jax.random.normal(ks[14], (N_LAYERS_B, B_I), f32)
    b_w_out = jax.random.normal(ks[15], (N_LAYERS_B, B_I, D_MODEL), f32) * B_I ** -0.5
    return {'x': x, 'meta_tokens': meta_tokens, 'norm_gains': norm_gains, 'ffn_w_in': ffn_w_in,
            'ffn_w_out': ffn_w_out, 'a_w_in': a_w_in, 'a_conv_w': a_conv_w, 'a_conv_b': a_conv_b,
            'a_gate_b': a_gate_b, 'a_w_out': a_w_out, 'b_w_in': b_w_in, 'b_f_bias': b_f_bias,
            'b_lb_raw': b_lb_raw, 'b_g_norm': b_g_norm, 'b_w_out': b_w_out}


def reference(x, meta_tokens, norm_gains, ffn_w_in, ffn_w_out, a_w_in, a_conv_w, a_conv_b, a_gate_b,
              a_w_out, b_w_in, b_f_bias, b_lb_raw, b_g_norm, b_w_out):
    bsz = x.shape[0]
    meta = jnp.broadcast_to(meta_tokens[None].astype(x.dtype), (bsz, N_META, D_MODEL))
    h = jnp.concatenate([meta, x], axis=1)
    sm = jax.nn.softmax(b_lb_raw.astype(jnp.float32), axis=0)
    lb_all = jnp.cumsum(sm, axis=0) - sm[0]
    for i in range(DEPTH):
        gn = norm_gains[i]
        h = h + 0.5 * rmsnorm(swiglu(rmsnorm(h, gn[0]), ffn_w_in[i, 0], ffn_w_out[i, 0]), gn[1])
        u = rmsnorm(h, gn[2])
        j = i // N_MIXERS
        if i % N_MIXERS == 0:
            y = mlstm_mixer(u, a_w_in[j], a_conv_w[j], a_conv_b[j], a_gate_b[j], a_w_out[j])
        else:
            y = hgrn2_mixer(u, b_w_in[j], b_f_bias[j], lb_all[i], b_g_norm[j], b_w_out[j])
        h = h + rmsnorm(y, gn[3])
        h = h + 0.5 * rmsnorm(swiglu(rmsnorm(h, gn[4]), ffn_w_in[i, 1], ffn_w_out[i, 1]), gn[5])
    return h[:, N_META:]
```

```python
import contextlib
import numpy as np
import concourse.bass as bass
import concourse.mybir as mybir
from concourse.bass_utils import run_bass_kernel_spmd

F32 = mybir.dt.float32
BF16 = mybir.dt.bfloat16
AF = mybir.ActivationFunctionType
ALU = mybir.AluOpType

D = 1024
NCH = 8
SEQ = 2048
NMETA = 16
T = SEQ + NMETA
DFF = 2816
NF = DFF // 128
EPS = 1e-6
A_COLS = 3088
B_COLS = 4096

MAXV = 4000


class Instr:
    __slots__ = ("eng", "fn", "deps", "is_dma", "key", "needs_inc", "sem", "val", "ordn", "idx")

    def __init__(self, eng, fn, is_dma, key):
        self.eng = eng
        self.fn = fn
        self.deps = []
        self.is_dma = is_dma
        self.key = key
        self.needs_inc = is_dma
        self.sem = None
        self.val = None
        self.ordn = None
        self.idx = None


class Tracker:
    ENGS = ("pe", "act", "dve", "pool", "sp")

    def __init__(self, nc, stack):
        self.nc = nc
        self.stack = stack
        self.q = {e: [] for e in self.ENGS}
        self.lastw = {}
        self.readers = {}
        self.nsem = 0

    def _newsem(self, name):
        self.nsem += 1
        return self.stack.enter_context(self.nc.semaphore(f"s{self.nsem}_{name}"))

    def add(self, eng, fn, reads=(), writes=(), dma_key=None):
        ins = Instr(eng, fn, dma_key is not None, dma_key)
        ins.idx = len(self.q[eng])
        deps = []
        for r in reads:
            for w in self.lastw.get(r, ()):
                deps.append((w, "raw"))
        for wr in writes:
            for w in self.lastw.get(wr, ()):
                deps.append((w, "waw"))
            for rd in self.readers.get(wr, ()):
                deps.append((rd, "war"))
        seen = set()
        for d, kind in deps:
            if d is ins or id(d) in seen:
                continue
            if (not d.is_dma) and (not ins.is_dma) and d.eng == eng:
                if eng == "pe":
                    continue
            seen.add(id(d))
            ins.deps.append(d)
            d.needs_inc = True
        for r in reads:
            self.readers.setdefault(r, []).append(ins)
        for wr in writes:
            self.lastw[wr] = [ins]
            self.readers[wr] = []
        self.q[eng].append(ins)
        return ins

    def alias(self, new_tokens, old_tokens):
        oldw, oldr = [], []
        for o in old_tokens:
            oldw.extend(self.lastw.get(o, ()))
            oldr.extend(self.readers.get(o, ()))
        for n in new_tokens:
            self.lastw[n] = list(oldw)
            self.readers[n] = list(oldr)

    def pe(self, fn, reads=(), writes=()):
        return self.add("pe", fn, reads, writes)

    def act(self, fn, reads=(), writes=()):
        return self.add("act", fn, reads, writes)

    def dve(self, fn, reads=(), writes=()):
        return self.add("dve", fn, reads, writes)

    def pool(self, fn, reads=(), writes=()):
        return self.add("pool", fn, reads, writes)

    def dma(self, eng, fn, key, reads=(), writes=()):
        return self.add(eng, fn, reads, writes, dma_key=key)

    def finalize_and_emit(self):
        nc = self.nc
        for e in self.ENGS:
            cnt = 0
            ordn = 0
            sem = None
            for ins in self.q[e]:
                if ins.is_dma or not ins.needs_inc:
                    continue
                if sem is None or cnt >= MAXV:
                    sem = self._newsem(e)
                    cnt = 0
                cnt += 1
                ordn += 1
                ins.sem, ins.val, ins.ordn = sem, cnt, ordn
        dstate = {}
        for e in self.ENGS:
            pass
        for ins in self._all_dma_in_order():
            st = dstate.get(ins.key)
            if st is None or st[1] + 16 > MAXV * 4:
                st = [self._newsem("d"), 0]
                dstate[ins.key] = st
            st[1] += 16
            ins.sem, ins.val = st[0], st[1]

        handles = {"pe": None, "act": None, "dve": None, "pool": None, "sp": None}
        tracker = self

        def emit_queue(e, h):
            waited_ord = {}
            waited_sem = {}
            for ins in tracker.q[e]:
                for d in ins.deps:
                    if d.is_dma:
                        k = id(d.sem)
                        if waited_sem.get(k, 0) >= d.val:
                            continue
                        waited_sem[k] = d.val
                        h.wait_ge(d.sem, d.val)
                    else:
                        if waited_ord.get(d.eng, 0) >= d.ordn:
                            continue
                        waited_ord[d.eng] = d.ordn
                        h.wait_ge(d.sem, d.val)
                bi = ins.fn(h)
                if ins.needs_inc:
                    bi.then_inc(ins.sem, 16 if ins.is_dma else 1)

        with nc.Block() as block:
            @block.tensor
            def _(h):
                emit_queue("pe", h)

            @block.scalar
            def _(h):
                emit_queue("act", h)

            @block.vector
            def _(h):
                emit_queue("dve", h)

            @block.gpsimd
            def _(h):
                emit_queue("pool", h)

            @block.sync
            def _(h):
                emit_queue("sp", h)

    def _all_dma_in_order(self):
        return self._dma_list

    _dma_list = None


def _patch_tracker():
    orig_add = Tracker.add

    def add(self, eng, fn, reads=(), writes=(), dma_key=None):
        ins = orig_add(self, eng, fn, reads, writes, dma_key)
        if dma_key is not None:
            if self._dma_list is None:
                self._dma_list = []
            self._dma_list.append(ins)
        return ins

    Tracker.add = add


_patch_tracker()


class Builder:
    def __init__(self, nseq=2, stages=("ffn_a0", "mlstm", "ffn_b0", "ffn_a1", "hgrn2", "ffn_b1"), dbg=False):
        self.nseq = nseq
        self.stages = stages
        self.nc = bass.Bass("TRN2", target_bir_lowering=False)
        self.stack = contextlib.ExitStack()
        self.T = Tracker(self.nc, self.stack)
        self._uid = 0

    def sb(self, name, shape, dt):
        return self.stack.enter_context(self.nc.sbuf_tensor(name, list(shape), dt))

    def ps(self, name, shape, dt=F32):
        return self.stack.enter_context(self.nc.psum_tensor(name, list(shape), dt))

    def build(self):
        nc = self.nc
        nseq = self.nseq
        self.x = nc.dram_tensor("x", [nseq, SEQ, D], F32, kind="ExternalInput").ap()
        self.meta = nc.dram_tensor("meta", [NMETA, D], F32, kind="ExternalInput").ap()
        self.pvec = nc.dram_tensor("pvec", [256, 128], F32, kind="ExternalInput").ap()
        self.gateb = nc.dram_tensor("gateb", [1, 16], F32, kind="ExternalInput").ap()
        self.ffn_w_in = nc.dram_tensor("ffn_w_in", [2, 2, D, 2 * DFF], F32, kind="ExternalInput").ap()
        self.ffn_w_out = nc.dram_tensor("ffn_w_out", [2, 2, DFF, D], F32, kind="ExternalInput").ap()
        self.a_w_in = nc.dram_tensor("a_w_in", [D, A_COLS], F32, kind="ExternalInput").ap()
        self.a_w_out = nc.dram_tensor("a_w_out", [D, D], F32, kind="ExternalInput").ap()
        self.b_w_in = nc.dram_tensor("b_w_in", [D, B_COLS], F32, kind="ExternalInput").ap()
        self.b_w_out = nc.dram_tensor("b_w_out", [D, D], F32, kind="ExternalInput").ap()
        self.out = nc.dram_tensor("out", [nseq, SEQ, D], F32, kind="ExternalOutput").ap()

        self.h = self.sb("h", [128, NCH, T], F32)
        self.NSLOT = 4
        self.wslot = [self.sb(f"wslot{i}", [128, 8, 512], BF16) for i in range(self.NSLOT)]
        self.wslot_next = 0
        self.ubuf = [self.sb(f"ubuf{i}", [128, NCH, 512], BF16) for i in range(2)]
        self.u_next = 0
        self.ubuf_small = self.sb("ubuf_small", [128, NCH, 16], BF16)
        AW = 7200
        self.arena = self.sb("arena", [128, AW], F32)
        ar = self.arena
        self.abuf = ar[:, 0:5632].bitcast(BF16).rearrange("p (f t) -> p f t", f=NF)
        self.sg = [ar[:, 5632 + i * 512: 5632 + (i + 1) * 512] for i in range(2)]
        self.abuf2 = ar[:, 6656:6832].bitcast(BF16).rearrange("p (f t) -> p f t", f=NF)
        self.qk = ar[:, 0:2048].bitcast(BF16).rearrange("p (c t) -> p c t", c=8)
        self.Vext = ar[:, 2048:4112].bitcast(BF16).rearrange("p (j h v) -> p j h v", j=4, h=8)
        self.zbuf = [ar[:, 4112 + i * 516: 4112 + i * 516 + 515] for i in range(2)]
        self.hsg = ar[:, 5144:7192].bitcast(BF16).rearrange("p (c t) -> p c t", c=8)
        self.arena_tokens = []
        self.PTa_s = None
        self.yt = self.sb("yt", [128, NCH, 512], F32)
        self.sq = self.sb("sq", [128, NCH, 512], BF16)
        self.yt2 = self.sb("yt2", [128, NCH, 16], F32)
        self.sq2 = self.sb("sq2", [128, NCH, 16], BF16)
        self.epsc = self.sb("epsc", [128, 1], F32)
        self.rstd = [self.sb(f"rstd{i}", [128, 512], F32) for i in range(2)]
        self.rstd_next = 0
        self.sgo = self.sb("sgo", [128, NCH, 512], BF16)
        self.arena2 = self.sb("arena2", [128, 3200], F32)
        a2 = self.arena2

        def carve(off, shape, dt):
            nel = int(np.prod(shape))
            words = nel if dt == F32 else (nel + 1) // 2
            v = a2[:, off:off + words]
            if dt != F32:
                v = v.bitcast(dt)
            if len(shape) == 2:
                names = "a b"
            elif len(shape) == 3:
                names = "a b c"
            else:
                names = "a b c d"
            if len(shape) > 1:
                kw = {nm: sz for nm, sz in zip(names.split(), shape)}
                v = v.rearrange("p (" + names + ") -> p " + names, **kw)
            return v
        self.lrep = carve(0, [8, 64], F32)
        self.bexp = [carve(512 + i * 128, [128], F32) for i in range(2)]
        self.qt = [carve(768 + i * 64, [128], BF16) for i in range(2)]
        self.Ka = [carve(896 + i * 64, [128], BF16) for i in range(2)]
        self.dn = [carve(1152 + i * 128, [128], F32) for i in range(2)]
        self.t1 = [carve(1408 + i * 128, [128], F32) for i in range(2)]
        self.Cst = carve(1664, [4, 129], F32)
        self.Cd = carve(2180, [129], F32)
        self.Cbf = carve(2312, [4, 128], BF16)
        self.nrep = carve(2568, [4, 128], BF16)
        self.halo = carve(2824, [8, 3], F32)
        self.gsb = carve(2848, [4, 16], F32)
        self.lneg = carve(2912, [4, 8], F32)
        self.avec = carve(2944, [4, 8], F32)
        self.gtmp = carve(2976, [4, 8], F32)
        self.qt_all = carve(0, [4, 128], BF16)
        self.kt_all = carve(256, [4, 128], BF16)
        self.Ka_all = carve(512, [4, 128], BF16)
        self.mPT = carve(768, [8, 128], BF16)
        self.nla = self.avec
        self.Sst = carve(0, [8, 128], F32)
        self.Sbf = carve(1024, [8, 128], BF16)
        self.qtl = [carve(1536 + i * 256, [512], BF16) for i in range(2)]
        self.ktl = [carve(2048 + i * 256, [512], BF16) for i in range(2)]
        self.kTs = [carve(2560 + i * 64, [128], BF16) for i in range(2)]
        self.decs = [carve(2688 + i * 8, [8], F32) for i in range(2)]
        self.Sd = carve(2704, [128], F32)
        self.PTa = carve(1536, [8, 128], BF16)
        self.kTa = carve(2048, [8, 128], BF16)
        self.decs_all = carve(2560, [8, 8], F32)
        self.arena2_tokens = []
        self.Vb = ar[:, 0:2048].bitcast(BF16).rearrange("p (j v) -> p j v", j=4)
        self.hsgb = ar[:, 2048:4096].bitcast(BF16).rearrange("p (c t) -> p c t", c=8)
        self.X = [ar[:, 2048 + i * 512: 2048 + (i + 1) * 512] for i in range(10)]
        self.PT = [self.sb(f"PT{i}", [128, 128], BF16) for i in range(2)]
        self.maskBD = self.sb("maskBD", [128, 128], F32)
        self.lbv = self.sb("lbv", [128, 8], F32)
        self.oml = self.sb("oml", [128, 8], F32)
        self.ones_f = self.sb("ones_f", [128, 64], F32)
        self.maskT = self.sb("maskT", [128, 128], F32)
        self.identb = self.sb("identb", [128, 128], BF16)
        self.gateb_bc = self.sb("gateb_bc", [128, 16], F32)
        self.sg_next = 0
        self.tmp = [self.sb(f"tmp{i}", [128, 512], F32) for i in range(2)]
        self.tmp_next = 0
        self.ptmp_next = 0
        self.xin = [self.sb(f"xin{i}", [128, D], F32) for i in range(2)]
        self.xin_next = 0
        self.ident = self.sb("ident", [128, 128], F32)
        self.ones_bf = self.sb("ones_bf", [128, 128], BF16)
        self.pv_raw = self.sb("pv_raw", [128, 2, 128], F32)
        self.pv = self.sb("pv", [128, 256], F32)
        self.gnh = self.sb("gnh", [128, 2, 6, NCH], F32)
        self.psum = [self.ps(f"psb{i}", [128, 512]) for i in range(8)]
        self.psum2_bf = self.psum[2][:, 0:512].bitcast(BF16)
        self.psum6_bf = self.psum[6][:, 0:512].bitcast(BF16)

        self.prologue()
        for s in range(nseq):
            self.load_seq(s)
            for st in self.stages:
                if st.startswith("ffn"):
                    layer = int(st[-1])
                    which = 0 if st[4] == "a" else 1
                    self.ffn_stage(layer, which)
                elif st == "mlstm":
                    self.mlstm_stage()
                elif st == "hgrn2":
                    self.hgrn2_stage()
            self.store_seq(s)
        self.T.finalize_and_emit()
        return nc

    def gain(self, layer, j, c):
        r = (layer * 6 + j) * 8 + c
        return self.pv[:, r:r + 1]

    def uid(self):
        self._uid += 1
        return self._uid

    def prologue(self):
        T_ = self.T
        nc = self.nc
        ident, ones_bf = self.ident, self.ones_bf
        T_.pool(lambda e: e.memset(ident[:], 0.0), writes=["ident"])
        T_.pool(lambda e: e.affine_select(out=ident[:], in_=ident[:], compare_op=ALU.not_equal, fill=1.0,
                                          base=0, pattern=[[-1, 128]], channel_multiplier=1),
                reads=["ident"], writes=["ident"])
        T_.pool(lambda e: e.memset(ones_bf[:], 1.0), writes=["ones"])
        T_.pool(lambda e: e.memset(self.arena[:], 0.0), writes=["arena_init"])
        self.arena_tokens = ["arena_init"]
        T_.pool(lambda e: e.memset(self.epsc[:], EPS), writes=["epsc"])
        maskT, identb = self.maskT, self.identb
        T_.pool(lambda e: e.memset(maskT[:], 1.0), writes=["maskT"])
        T_.pool(lambda e: e.affine_select(out=maskT[:], in_=maskT[:], compare_op=ALU.is_ge, fill=0.0,
                                          base=0, pattern=[[1, 128]], channel_multiplier=-1),
                reads=["maskT"], writes=["maskT"])
        T_.pool(lambda e: e.tensor_copy(identb[:], ident[:]), reads=["ident"], writes=["identb"])
        T_.pool(lambda e: e.tensor_copy(self.maskBD[:], maskT[:]), reads=["maskT"], writes=["maskBD"])
        T_.pool(lambda e: e.memset(self.maskBD[0:64, 64:128], 0.0), reads=["maskBD"], writes=["maskBD"])
        T_.pool(lambda e: e.memset(self.ones_f[:], 1.0), writes=["ones_f"])
        T_.dma("sp", lambda e: e.dma_start(out=self.gateb_bc[:], in_=self.gateb.partition_broadcast(128)),
               key="gateb", writes=["gateb"])
        pv_raw, pv = self.pv_raw, self.pv
        T_.dma("sp", lambda e: e.dma_start(out=pv_raw[:], in_=self.pvec.rearrange("(a p) f -> p a f", p=128)),
               key="pv_raw", writes=["pv_raw"])
        for a in range(2):
            pst = self.psum[a]
            T_.pe(lambda e, a=a, pst=pst: e.transpose(pst[:, 0:128], pv_raw[:, a, :], ident[:]),
                  reads=["pv_raw", "ident"], writes=[("ps", a)])
            T_.dve(lambda e, a=a, pst=pst: e.tensor_copy(pv[:, a * 128:(a + 1) * 128], pst[:, 0:128]),
                   reads=[("ps", a)], writes=["pv"])
        gnh = self.gnh
        T_.dve(lambda e: e.tensor_scalar(gnh[:].rearrange("p l j c -> p (l j c)"), pv[:, 0:96], 0.5, None, ALU.mult),
               reads=["pv"], writes=["gnh"])

    def load_seq(self, s):
        T_ = self.T
        h = self.h
        ident = self.ident
        nblk = SEQ // 128
        for tb in range(-1, nblk):
            xi = self.xin_next
            self.xin_next ^= 1
            xin = self.xin[xi]
            if tb < 0:
                np_, col0 = NMETA, 0
                src = self.meta[:, :]
            else:
                np_, col0 = 128, NMETA + tb * 128
                src = self.x[s, tb * 128:(tb + 1) * 128, :]
            T_.dma("sp", lambda e, xin=xin, src=src, np_=np_: e.dma_start(out=xin[0:np_, :], in_=src),
                   key=("xin", xi), writes=[("xin", xi)])
            for half in range(2):
                pb = 6 + half
                pst = self.psum[pb]
                for cc in range(4):
                    c = half * 4 + cc
                    T_.pe(lambda e, pst=pst, cc=cc, c=c, xin=xin, np_=np_:
                          e.transpose(pst[:, cc * 128:cc * 128 + np_], xin[0:np_, c * 128:(c + 1) * 128],
                                      ident[0:np_, 0:np_]),
                          reads=[("xin", xi), "ident"], writes=[("ps", pb)])
                eng = T_.dve if half == 0 else T_.act
                if half == 0:
                    T_.dve(lambda e, pst=pst, half=half, col0=col0, np_=np_:
                           e.tensor_copy(h[:, half * 4:half * 4 + 4, col0:col0 + np_],
                                         pst[:].rearrange("p (c t) -> p c t", c=4)[:, :, 0:np_]),
                           reads=[("ps", pb)], writes=[("h", c_, "io") for c_ in range(half * 4, half * 4 + 4)])
                else:
                    T_.act(lambda e, pst=pst, half=half, col0=col0, np_=np_:
                           e.activation(out=h[:, half * 4:half * 4 + 4, col0:col0 + np_],
                                        in_=pst[:].rearrange("p (c t) -> p c t", c=4)[:, :, 0:np_], func=AF.Copy),
                           reads=[("ps", pb)], writes=[("h", c_, "io") for c_ in range(half * 4, half * 4 + 4)])
        self.h_io_to_tiles()

    TILES = [(0, 512), (512, 512), (1024, 512), (1536, 512), (2048, 16)]

    def htok(self, c, ti):
        return ("h", c, ti)

    def h_io_to_tiles(self):
        self.T.alias([self.htok(c, ti) for c in range(NCH) for ti in range(len(self.TILES))],
                     [("h", c, "io") for c in range(NCH)])

    def h_tiles_to_io(self):
        self.T.alias([("h", c, "io") for c in range(NCH)],
                     [self.htok(c, ti) for c in range(NCH) for ti in range(len(self.TILES))])

    def store_seq(self, s):
        T_ = self.T
        h = self.h
        ident = self.ident
        self.h_tiles_to_io()
        nblk = SEQ // 128
        for tb in range(nblk):
            xi = self.xin_next
            self.xin_next ^= 1
            xout = self.xin[xi]
            col0 = NMETA + tb * 128
            for half in range(2):
                pb = 6 + half
                pst = self.psum[pb]
                for cc in range(4):
                    c = half * 4 + cc
                    T_.pe(lambda e, pst=pst, cc=cc, c=c, col0=col0:
                          e.transpose(pst[:, cc * 128:(cc + 1) * 128], h[:, c, col0:col0 + 128], ident[:]),
                          reads=["ident"], writes=[("ps", pb), ("h", c, "io")])
                if half == 0:
                    T_.dve(lambda e, pst=pst, xout=xout, half=half:
                           e.tensor_copy(xout[:, half * 512:(half + 1) * 512], pst[:]),
                           reads=[("ps", pb)], writes=[("xin", xi)])
                else:
                    T_.act(lambda e, pst=pst, xout=xout, half=half:
                           e.activation(out=xout[:, half * 512:(half + 1) * 512], in_=pst[:], func=AF.Copy),
                           reads=[("ps", pb)], writes=[("xin", xi)])
            T_.dma("sp", lambda e, xout=xout, s=s, tb=tb:
                   e.dma_start(out=self.out[s, tb * 128:(tb + 1) * 128, :], in_=xout[:]),
                   key=("xin", xi), reads=[("xin", xi)], writes=[("out", s, tb)])
        T_.add("sp", lambda e: e.nop(), reads=[("out", s, tb) for tb in range(nblk)])

    def wload(self, src_ap, nk, ncols, slot=None):
        if slot is None:
            si = self.wslot_next
            self.wslot_next = (si + 1) % self.NSLOT
        else:
            si = slot
        slot = self.wslot[si]
        tok = ("w", si)
        src = src_ap.rearrange("(k p) f -> p k f", p=128)
        self.T.dma("pool", lambda e, slot=slot, src=src, nk=nk, ncols=ncols:
                   e.dma_start(out=slot[:, 0:nk, 0:ncols], in_=src), key=tok, writes=[tok])
        return slot, tok

    def wplan_reset(self, plan):
        self._wplan = plan
        self._wgot = []

    def wget(self, idx):
        while len(self._wgot) < min(len(self._wplan), idx + 3):
            src, nk, ncols = self._wplan[len(self._wgot)]
            self._wgot.append(self.wload(src, nk, ncols))
        return self._wgot[idx]

    def rstd_from_sq(self, sq_tokens, n, psb, dim_chunks=NCH):
        T_ = self.T
        pst = self.psum[psb]
        sq, ones = self.sq, self.ones_bf
        for c in range(dim_chunks):
            T_.pe(lambda e, c=c: e.matmul(pst[:, 0:n], lhsT=ones[:], rhs=sq[:, c, 0:n],
                                          start=(c == 0), stop=(c == dim_chunks - 1)),
                  reads=["ones", sq_tokens[c]], writes=[("ps", psb)])
        ri = self.rstd_next
        self.rstd_next ^= 1
        rs = self.rstd[ri]
        rtok = ("rstd", ri)
        T_.act(lambda e: e.activation(out=rs[:, 0:n], in_=pst[:, 0:n], func=AF.Ln,
                                      scale=1.0 / (128 * dim_chunks), bias=self.eps_ap()),
               reads=[("ps", psb), "epsc"], writes=[rtok])
        T_.act(lambda e: e.activation(out=rs[:, 0:n], in_=rs[:, 0:n], func=AF.Exp, scale=-0.5), reads=[rtok], writes=[rtok])
        return rs, rtok

    def eps_ap(self):
        return self.epsc[:, 0:1]

    def prenorm(self, layer, j, ti, psb):
        T_ = self.T
        h, sq = self.h, self.sq
        t0, n = self.TILES[ti]
        for c in range(NCH):
            T_.act(lambda e, c=c: e.activation(out=sq[:, c, 0:n], in_=h[:, c, t0:t0 + n], func=AF.Square),
                   reads=[self.htok(c, ti)], writes=[("sq", c)])
        rs, rtok = self.rstd_from_sq([("sq", c) for c in range(NCH)], n, psb)
        if n <= 16:
            ui, ub = 2, self.ubuf_small
        else:
            ui = self.u_next
            self.u_next ^= 1
            ub = self.ubuf[ui]
        for c in range(NCH):
            T_.dve(lambda e, c=c: e.scalar_tensor_tensor(out=ub[:, c, 0:n], in0=h[:, c, t0:t0 + n],
                                                         scalar=self.gain(layer, j, c), in1=rs[:, 0:n],
                                                         op0=ALU.mult, op1=ALU.mult),
                   reads=[self.htok(c, ti), rtok, "pv"], writes=[("u", ui, c)])
        return ub, ui

    def postnorm_residual(self, ti, psb):
        T_ = self.T
        h, yt = self.h, self.yt
        t0, n = self.TILES[ti]
        rs, rtok = self.rstd_from_sq([("sq", c) for c in range(NCH)], n, psb)
        for c in range(NCH):
            tix = self.tmp_next
            self.tmp_next ^= 1
            tmp = self.tmp[tix]
            T_.dve(lambda e, c=c, tmp=tmp: e.tensor_tensor(out=tmp[:, 0:n], in0=yt[:, c, 0:n], in1=rs[:, 0:n], op=ALU.mult),
                   reads=[("yt", c), rtok], writes=[("tmp", tix)])
            T_.dve(lambda e, c=c, tmp=tmp: e.tensor_tensor(out=h[:, c, t0:t0 + n], in0=h[:, c, t0:t0 + n], in1=tmp[:, 0:n], op=ALU.add),
                   reads=[("tmp", tix), self.htok(c, ti)], writes=[self.htok(c, ti)])

    def arena_handoff(self, new_tokens):
        self.T.alias(new_tokens, self.arena_tokens)
        self.arena_tokens = list(new_tokens)

    def arena2_handoff(self, new_tokens):
        self.T.alias(new_tokens, self.arena2_tokens)
        self.arena2_tokens = list(new_tokens)

    def ffn_stage(self, layer, which):
        T_ = self.T
        self.arena_handoff([("a", fl, f) for fl in (True, False) for f in range(NF)] + [("sg", 0), ("sg", 1)])
        j_pre, j_post = (0, 1) if which == 0 else (4, 5)
        w_in = self.ffn_w_in[layer, which]
        w_out = self.ffn_w_out[layer, which]
        abuf, yt, sq = self.abuf, self.yt, self.sq
        ntile = len(self.TILES)
        groups = [[i] for i in range(ntile)]
        if ntile > 1 and self.TILES[-1][1] <= 16:
            groups = groups[:-2] + [[ntile - 2, ntile - 1]]
        plan = []
        for _g in groups:
            for js in range(6):
                nf_ = 4 if js < 5 else 2
                plan.append((w_in[:, js * 512: js * 512 + nf_ * 128], 8, nf_ * 128))
                plan.append((w_in[:, DFF + js * 512: DFF + js * 512 + nf_ * 128], 8, nf_ * 128))
            for dh_ in range(2):
                for ks_ in range(3):
                    nk_ = 8 if ks_ < 2 else 6
                    plan.append((w_out[ks_ * 1024: ks_ * 1024 + nk_ * 128, dh_ * 512:(dh_ + 1) * 512], nk_, 512))
        self.wplan_reset(plan)
        ubs = {}
        for ti in groups[0]:
            ubs[ti] = self.prenorm(layer, j_pre, ti, psb=5)
        pending_post = []
        for gi, grp in enumerate(groups):
            wbase = 18 * gi
            for jslab in range(6):
                if jslab == 1:
                    for g_ in pending_post:
                        next(g_, None)
                if jslab == 3 and gi + 1 < len(groups):
                    for ti in groups[gi + 1]:
                        ubs[ti] = self.prenorm(layer, j_pre, ti, psb=5)
                nf = 4 if jslab < 5 else 2
                gs, gtok = self.wget(wbase + 2 * jslab)
                us, utok = self.wget(wbase + 2 * jslab + 1)
                for fi in range(nf):
                    f = jslab * 4 + fi
                    if jslab >= 1 and pending_post:
                        if next(pending_post[0], "done") == "done":
                            pending_post.pop(0)
                            if pending_post:
                                next(pending_post[0], None)
                    for ti in grp:
                        t0, n = self.TILES[ti]
                        ub, ui = ubs[ti]
                        par = self.uid() % 2
                        gb, ubk = 0 + par * 2, 1 + par * 2
                        gps, ups = self.psum[gb], self.psum[ubk]
                        for k in range(8):
                            T_.pe(lambda e, k=k, fi=fi, gs=gs, ub=ub, n=n, gps=gps:
                                  e.matmul(gps[:, 0:n], lhsT=gs[:, k, fi * 128:(fi + 1) * 128], rhs=ub[:, k, 0:n],
                                           start=(k == 0), stop=(k == 7)),
                                  reads=[gtok, ("u", ui, k)], writes=[("ps", gb)])
                        for k in range(8):
                            T_.pe(lambda e, k=k, fi=fi, us=us, ub=ub, n=n, ups=ups:
                                  e.matmul(ups[:, 0:n], lhsT=us[:, k, fi * 128:(fi + 1) * 128], rhs=ub[:, k, 0:n],
                                           start=(k == 0), stop=(k == 7)),
                                  reads=[utok, ("u", ui, k)], writes=[("ps", ubk)])
                        sgi = self.sg_next
                        self.sg_next ^= 1
                        sg = self.sg[sgi]
                        T_.act(lambda e, sg=sg, gps=gps, n=n: e.activation(out=sg[:, 0:n], in_=gps[:, 0:n], func=AF.Silu),
                               reads=[("ps", gb)], writes=[("sg", sgi)])
                        acol = 0 if ti == grp[0] else 512 - 16
                        ab = abuf if ti == grp[0] else self.abuf2
                        T_.dve(lambda e, sg=sg, ups=ups, n=n, f=f, ab=ab:
                               e.tensor_tensor(out=ab[:, f, 0:n], in0=sg[:, 0:n], in1=ups[:, 0:n], op=ALU.mult),
                               reads=[("sg", sgi), ("ps", ubk)], writes=[("a", ti == grp[0], f)])
            for dh in range(2):
                ybank = {}
                for ks in range(3):
                    nk = 8 if ks < 2 else 6
                    ws, wtok = self.wget(wbase + 12 + dh * 3 + ks)
                    for ti in grp:
                        t0, n = self.TILES[ti]
                        ab = abuf if ti == grp[0] else self.abuf2
                        for di in range(4):
                            yb = 4 + di if ti == grp[0] else di
                            yps = self.psum[yb]
                            for kk in range(nk):
                                fch = ks * 8 + kk
                                T_.pe(lambda e, kk=kk, di=di, ws=ws, ab=ab, fch=fch, n=n, yps=yps, ks=ks, nk=nk:
                                      e.matmul(yps[:, 0:n], lhsT=ws[:, kk, di * 128:(di + 1) * 128], rhs=ab[:, fch, 0:n],
                                               start=(ks == 0 and kk == 0), stop=(ks == 2 and kk == nk - 1)),
                                      reads=[wtok, ("a", ti == grp[0], fch)], writes=[("ps", yb)])
                for ti in grp:
                    t0, n = self.TILES[ti]
                    first = ti == grp[0]
                    ytt = yt if first else self.yt2
                    sqq = sq if first else self.sq2
                    for di in range(4):
                        d = dh * 4 + di
                        yb = 4 + di if first else di
                        yps = self.psum[yb]
                        T_.act(lambda e, d=d, yps=yps, n=n, sqq=sqq: e.activation(out=sqq[:, d, 0:n], in_=yps[:, 0:n], func=AF.Square),
                               reads=[("ps", yb)], writes=[("sq" if first else "sq2", d)])
                        T_.act(lambda e, d=d, yps=yps, n=n, ytt=ytt: e.activation(out=ytt[:, d, 0:n], in_=yps[:, 0:n], func=AF.Copy,
                                                                         scale=self.gnh[:, layer, j_post, d:d + 1]),
                               reads=[("ps", yb), "gnh"], writes=[("yt" if first else "yt2", d)])
            for ti in grp:
                first = ti == grp[0]
                pending_post.append(self.postnorm_gen(ti, 5, first))
        for g_ in pending_post:
            for _ in g_:
                pass

    def postnorm_residual2(self, ti, psb, first):
        for _ in self.postnorm_gen(ti, psb, first):
            pass

    def postnorm_gen(self, ti, psb, first):
        T_ = self.T
        h = self.h
        yt = self.yt if first else self.yt2
        sq = self.sq if first else self.sq2
        sqn = "sq" if first else "sq2"
        ytn = "yt" if first else "yt2"
        t0, n = self.TILES[ti]
        pst = self.psum[psb]
        ones = self.ones_bf
        for c in range(NCH):
            T_.pe(lambda e, c=c: e.matmul(pst[:, 0:n], lhsT=ones[:], rhs=sq[:, c, 0:n],
                                          start=(c == 0), stop=(c == NCH - 1)),
                  reads=["ones", (sqn, c)], writes=[("ps", psb)])
        ri = self.rstd_next
        self.rstd_next ^= 1
        rs = self.rstd[ri]
        rtok = ("rstd", ri)
        T_.act(lambda e: e.activation(out=rs[:, 0:n], in_=pst[:, 0:n], func=AF.Ln,
                                      scale=1.0 / D, bias=self.eps_ap()),
               reads=[("ps", psb), "epsc"], writes=[rtok])
        T_.act(lambda e: e.activation(out=rs[:, 0:n], in_=rs[:, 0:n], func=AF.Exp, scale=-0.5), reads=[rtok], writes=[rtok])
        yield
        for c in range(NCH):
            tix = self.tmp_next
            self.tmp_next ^= 1
            tmp = self.tmp[tix]
            T_.dve(lambda e, c=c, tmp=tmp: e.tensor_tensor(out=tmp[:, 0:n], in0=yt[:, c, 0:n], in1=rs[:, 0:n], op=ALU.mult),
                   reads=[(ytn, c), rtok], writes=[("tmp", tix)])
            T_.dve(lambda e, c=c, tmp=tmp: e.tensor_tensor(out=h[:, c, t0:t0 + n], in0=h[:, c, t0:t0 + n], in1=tmp[:, 0:n], op=ALU.add),
                   reads=[("tmp", tix), self.htok(c, ti)], writes=[self.htok(c, ti)])
            yield

    def mlstm_stage(self):
        T_ = self.T
        w_in, w_out = self.a_w_in, self.a_w_out
        h, sq, yt = self.h, self.sq, self.yt
        qk, Vext, hsg, sgo = self.qk, self.Vext, self.hsg, self.sgo
        ident, identb, maskT, ones = self.ident, self.identb, self.maskT, self.ones_bf
        Cst, Cbf, nrep, Cd = self.Cst, self.Cbf, self.nrep, self.Cd
        gsb, lneg, avec, gtmp, lrep = self.gsb, self.lneg, self.avec, self.gtmp, self.lrep
        pv = self.pv
        self.arena_handoff([("qk", c) for c in range(8)] + [("V", j, hf) for j in range(4) for hf in range(2)]
                           + [("zb", i) for i in range(2)] + [("hsg", c) for c in range(8)])
        self.arena2_handoff(["Call", "Cbfall", "nrepall", ("qt_all", 0), ("kt_all", 0), ("Ka_all", 0), ("mPT", 0, 0), ("mPT", 0, 1)]
                            + [("halo", c) for c in range(8)]
                            + [(nm, j) for nm in ("gsb", "lneg", "nla", "gtmp") for j in range(4)])
        T_.alias([("EA", 0, 0), ("EA", 0, 1)], [("xin", 0)])
        T_.alias([("rep", 0, 0), ("rep", 0, 1)], [("xin", 1)])
        T_.dve(lambda e: e.memset(Cst[:], 0.0), writes=["Call"])
        T_.dve(lambda e: e.memset(Cbf[:], 0.0), writes=["Cbfall"])
        T_.dve(lambda e: e.memset(nrep[:], 0.0), writes=["nrepall"])
        T_.dve(lambda e: e.memset(self.halo[:], 0.0), writes=[("halo", c) for c in range(8)])
        T_.dve(lambda e: e.memset(Vext[:], 1.0), writes=[("V", j, hf) for j in range(4) for hf in range(2)])
        rot = {"zb": 0, "bexp": 0, "qt": 0, "Ka": 0, "PT": 0, "dn": 0, "t1": 0, "pp": 0, "pv": 0}

        def nxt(k, m=2):
            v = rot[k]
            rot[k] = (v + 1) % m
            return v

        for ti, (t0, n) in enumerate(self.TILES):
            self._mlstm_tile(ti, t0, n, nxt)
        T_.alias([("xin", 0)], [("EA", 0, 0), ("EA", 0, 1)])
        T_.alias([("xin", 1)], [("rep", 0, 0), ("rep", 0, 1)])

    def _mlstm_tile(self, ti, t0, n, nxt):
        T_ = self.T
        w_in, w_out = self.a_w_in, self.a_w_out
        h, sq, yt = self.h, self.sq, self.yt
        qk, Vext, hsg, sgo = self.qk, self.Vext, self.hsg, self.sgo
        ident, identb, maskT, ones = self.ident, self.identb, self.maskT, self.ones_bf
        Cst, Cbf, nrep, Cd = self.Cst, self.Cbf, self.nrep, self.Cd
        gsb, lneg, avec, gtmp, lrep = self.gsb, self.lneg, self.avec, self.gtmp, self.lrep
        pv = self.pv
        if True:
            ub, ui = self.prenorm(0, 2, ti, psb=5)
            utoks = [("u", ui, k) for k in range(8)]
            nchunk = (n + 127) // 128
            for part in range(2):
                slab, stok = self.wload(w_in[:, part * 512:(part + 1) * 512], 8, 512)
                for cc in range(4):
                    c8 = part * 4 + cc
                    pb = nxt("pp")
                    pst = self.psum[pb]
                    for k in range(8):
                        T_.pe(lambda e, k=k, cc=cc, slab=slab, pst=pst:
                              e.matmul(pst[:, 0:n], lhsT=slab[:, k, cc * 128:(cc + 1) * 128], rhs=ub[:, k, 0:n],
                                       start=(k == 0), stop=(k == 7)),
                              reads=[stok, utoks[k]], writes=[("ps", pb)])
                    zi = nxt("zb")
                    zb = self.zbuf[zi]
                    T_.act(lambda e, zb=zb, pst=pst: e.activation(out=zb[:, 3:3 + n], in_=pst[:, 0:n], func=AF.Copy),
                           reads=[("ps", pb)], writes=[("zb", zi)])
                    T_.act(lambda e, zb=zb, c8=c8: e.activation(out=zb[:, 0:3], in_=self.halo[:, c8, :], func=AF.Copy),
                           reads=[("halo", c8)], writes=[("zb", zi)])
                    T_.act(lambda e, zb=zb, c8=c8: e.activation(out=self.halo[:, c8, :], in_=zb[:, n:n + 3], func=AF.Copy),
                           reads=[("zb", zi)], writes=[("halo", c8)])
                    tix = self.tmp_next
                    self.tmp_next ^= 1
                    acc = self.tmp[tix]
                    wcol = lambda kk, c8=c8: pv[:, 96 + kk * 8 + c8: 96 + kk * 8 + c8 + 1]
                    bcol = pv[:, 128 + c8:128 + c8 + 1]
                    T_.dve(lambda e, zb=zb, acc=acc, wcol=wcol, bcol=bcol:
                           e.tensor_scalar(acc[:, 0:n], zb[:, 3:3 + n], wcol(3), bcol, ALU.mult, ALU.add),
                           reads=[("zb", zi), "pv"], writes=[("tmp", tix)])
                    for kk in (2, 1, 0):
                        T_.dve(lambda e, zb=zb, acc=acc, wcol=wcol, kk=kk:
                               e.scalar_tensor_tensor(out=acc[:, 0:n], in0=zb[:, kk:kk + n], scalar=wcol(kk),
                                                      in1=acc[:, 0:n], op0=ALU.mult, op1=ALU.add),
                               reads=[("zb", zi), ("tmp", tix), "pv"], writes=[("tmp", tix)])
                    T_.act(lambda e, acc=acc, c8=c8: e.activation(out=qk[:, c8, 0:n], in_=acc[:, 0:n], func=AF.Silu),
                           reads=[("tmp", tix)], writes=[("qk", c8)])
            for half in range(2):
                slab, stok = self.wload(w_in[:, 2048 + half * 512: 2048 + (half + 1) * 512], 8, 512)
                for cc in range(4):
                    c8 = half * 4 + cc
                    pb = nxt("pp")
                    pst = self.psum[pb]
                    for k in range(8):
                        T_.pe(lambda e, k=k, cc=cc, slab=slab, pst=pst:
                              e.matmul(pst[:, 0:n], lhsT=slab[:, k, cc * 128:(cc + 1) * 128], rhs=ub[:, k, 0:n],
                                       start=(k == 0), stop=(k == 7)),
                              reads=[stok, utoks[k]], writes=[("ps", pb)])
                    T_.act(lambda e, pst=pst, c8=c8: e.activation(out=sgo[:, c8, 0:n], in_=pst[:, 0:n], func=AF.Sigmoid),
                           reads=[("ps", pb)], writes=[("sgo", c8)])
            for half in range(2):
                slab, stok = self.wload(w_in[:, 1024 + half * 512: 1024 + (half + 1) * 512], 8, 512)
                for j in range(nchunk):
                    cn = min(128, n - j * 128)
                    pb = 2 + nxt("pv")
                    pst = self.psum[pb]
                    for k in range(8):
                        T_.pe(lambda e, k=k, j=j, cn=cn, slab=slab, pst=pst:
                              e.matmul(pst[0:cn, 0:512], lhsT=ub[:, k, j * 128:j * 128 + cn], rhs=slab[:, k, 0:512],
                                       start=(k == 0), stop=(k == 7)),
                              reads=[stok, utoks[k]], writes=[("ps", pb)])
                    T_.act(lambda e, j=j, cn=cn, half=half, pst=pst:
                           e.activation(out=Vext[0:cn, j, half * 4:(half + 1) * 4, 0:128],
                                        in_=pst[0:cn, :].rearrange("p (h v) -> p h v", h=4), func=AF.Copy),
                           reads=[("ps", pb)], writes=[("V", j, half)])
            gslab, gstok = self.wload(w_in[:, 3072:3088], 8, 16)
            for j in range(nchunk):
                cn = min(128, n - j * 128)
                pst = self.psum[3]
                for k in range(8):
                    T_.pe(lambda e, k=k, j=j, cn=cn, pst=pst:
                          e.matmul(pst[0:cn, j * 16:(j + 1) * 16], lhsT=ub[:, k, j * 128:j * 128 + cn], rhs=gslab[:, k, 0:16],
                                   start=(k == 0), stop=(k == 7)),
                          reads=[gstok, utoks[k]], writes=[("ps", 3)])
                T_.dve(lambda e, j=j, cn=cn, pst=pst:
                       e.tensor_tensor(out=gsb[0:cn, j, :], in0=pst[0:cn, j * 16:(j + 1) * 16], in1=self.gateb_bc[0:cn, :], op=ALU.add),
                       reads=[("ps", 3), "gateb"], writes=[("gsb", j)])
                T_.act(lambda e, j=j, cn=cn: e.activation(out=gtmp[0:cn, j, :], in_=gsb[0:cn, j, 8:16], func=AF.Exp, scale=-1.0),
                       reads=[("gsb", j)], writes=[("gtmp", j)])
                T_.act(lambda e, j=j, cn=cn: e.activation(out=lneg[0:cn, j, :], in_=gtmp[0:cn, j, :], func=AF.Ln, bias=1.0),
                       reads=[("gtmp", j)], writes=[("lneg", j)])
                T_.pe(lambda e, j=j, cn=cn, pst=pst: e.matmul(pst[0:cn, 64 + 8 * j:72 + 8 * j], lhsT=self.maskT[0:cn, 0:cn], rhs=lneg[0:cn, j, :],
                                                      start=True, stop=True),
                      reads=["maskT", ("lneg", j)], writes=[("ps", 3)])
                T_.dve(lambda e, j=j, cn=cn, pst=pst: e.scalar_tensor_tensor(out=self.nla[0:cn, j, :], in0=pst[0:cn, 64 + 8 * j:72 + 8 * j], scalar=-1.0,
                                                                     in1=gsb[0:cn, j, 0:8], op0=ALU.mult, op1=ALU.subtract),
                       reads=[("ps", 3), ("gsb", j)], writes=[("nla", j)])
            T_.alias(self._mlstm_set_tokens(1), [("yt", d) for d in range(NCH)])
            self._mlstm_front(0, min(128, n), 0, 0)
            for j in range(nchunk):
                if j + 1 < nchunk:
                    self._mlstm_front(j + 1, min(128, n - (j + 1) * 128), (j + 1) * 128, (j + 1) % 2)
                self._mlstm_back(j, min(128, n - j * 128), j * 128, j % 2)
            T_.alias([("yt", d) for d in range(NCH)], self._mlstm_set_tokens(1))
            self._mlstm_outproj(ti, n)

    def _mlstm_chunk(self, j, cn, c0, nxt):
        T_ = self.T
        qk, Vext, hsg, sgo = self.qk, self.Vext, self.hsg, self.sgo
        ident, identb, maskT, ones = self.ident, self.identb, self.maskT, self.ones_bf
        Cst, Cbf, nrep, Cd = self.Cst, self.Cbf, self.nrep, self.Cd
        gsb, lneg, avec, gtmp, lrep = self.gsb, self.lneg, self.avec, self.gtmp, self.lrep
        if True:
            if True:
                pst4 = self.psum[4]
                T_.pe(lambda e, j=j, cn=cn: e.matmul(pst4[0:cn, 64:72], lhsT=maskT[0:cn, 0:cn], rhs=lneg[0:cn, j, :], start=True, stop=True),
                      reads=["maskT", ("lneg", j)], writes=[("ps", 4)])
                T_.dve(lambda e, j=j, cn=cn: e.tensor_tensor(out=gtmp[0:cn, j, :], in0=pst4[0:cn, 64:72], in1=gsb[0:cn, j, 0:8], op=ALU.add),
                       reads=[("ps", 4), ("gsb", j)], writes=[("gtmp", j)])
                T_.act(lambda e, j=j, cn=cn: e.activation(out=avec[0:cn, j, :], in_=gtmp[0:cn, j, :], func=AF.Exp),
                       reads=[("gtmp", j)], writes=[("avec", j)])
                T_.dve(lambda e, j=j, cn=cn: e.tensor_copy(lrep[0:cn, :, :], lneg[0:cn, j, :].unsqueeze(2).to_broadcast([cn, 8, 64])),
                       reads=[("lneg", j)], writes=["lrep"])
                for hp in range(4):
                    self._mlstm_pair(j, cn, c0, hp, nxt)

    def _mlstm_pair(self, j, cn, c0, hp, nxt):
        T_ = self.T
        qk, Vext, hsg, sgo = self.qk, self.Vext, self.hsg, self.sgo
        ident, identb, maskT, ones = self.ident, self.identb, self.maskT, self.ones_bf
        Cst, Cbf, nrep, Cd = self.Cst, self.Cbf, self.nrep, self.Cd
        gsb, lneg, avec, gtmp, lrep = self.gsb, self.lneg, self.avec, self.gtmp, self.lrep
        if True:
            if True:
                if True:
                    fbb = hp % 2
                    fbp = self.psum[fbb]
                    T_.pe(lambda e, hp=hp, cn=cn, fbp=fbp: e.matmul(fbp[:, 0:cn], lhsT=lrep[0:cn, 2 * hp:2 * hp + 2, :].rearrange("p a b -> p (a b)"),
                                                           rhs=maskT[0:cn, 0:cn], start=True, stop=True),
                          reads=["lrep", "maskT"], writes=[("ps", fbb)])
                    bi = nxt("bexp")
                    bexp = self.bexp[bi]
                    T_.act(lambda e, bexp=bexp, cn=cn, fbp=fbp: e.activation(out=bexp[:, 0:cn], in_=fbp[:, 0:cn], func=AF.Exp, scale=-1.0),
                           reads=[("ps", fbb)], writes=[("bexp", bi)])
                    qi = nxt("qt")
                    qt = self.qt[qi]
                    T_.dve(lambda e, qt=qt, bexp=bexp, hp=hp, cn=cn, c0=c0:
                           e.scalar_tensor_tensor(out=qt[:, 0:cn], in0=qk[:, hp, c0:c0 + cn], scalar=0.125, in1=bexp[:, 0:cn],
                                                  op0=ALU.mult, op1=ALU.mult),
                           reads=[("qk", hp), ("bexp", bi)], writes=[("qt", qi)])
                    kps = self.psum2_bf[:, 0:128]
                    T_.pe(lambda e, hp=hp, cn=cn, c0=c0, kps=kps:
                          e.transpose(kps[0:cn, :], qk[:, 4 + hp, c0:c0 + cn], identb[:]),
                          reads=[("qk", 4 + hp), "identb"], writes=[("ps", 2)])
                    kai = nxt("Ka")
                    Ka = self.Ka[kai]
                    T_.dve(lambda e, Ka=Ka, kps=kps, hp=hp, j=j, cn=cn:
                           e.tensor_tensor(out=Ka[0:cn, :].rearrange("p (a b) -> p a b", a=2),
                                           in0=kps[0:cn, :].rearrange("p (a b) -> p a b", a=2),
                                           in1=avec[0:cn, j, 2 * hp:2 * hp + 2].unsqueeze(2).to_broadcast([cn, 2, 64]), op=ALU.mult),
                           reads=[("ps", 2), ("avec", j)], writes=[("Ka", kai)])
                    ups = self.psum[3]
                    for ev in range(2):
                        self._mlstm_head(j, cn, c0, hp, ev, nxt, qt, qi, Ka, kai, ups)
                    self._mlstm_state(hp, cn, bexp, bi, ups)

    def _mlstm_head(self, j, cn, c0, hp, ev, nxt, qt, qi, Ka, kai, ups):
        T_ = self.T
        qk, Vext, hsg, sgo = self.qk, self.Vext, self.hsg, self.sgo
        ident, identb, maskT, ones = self.ident, self.identb, self.maskT, self.ones_bf
        Cst, Cbf, nrep, Cd = self.Cst, self.Cbf, self.nrep, self.Cd
        avec = self.avec
        if True:
            if True:
                if True:
                    if True:
                        hh = 2 * hp + ev
                        r0 = 64 * ev
                        stb = 5 - ev
                        stp = self.psum[stb]
                        T_.pe(lambda e, hp=hp, cn=cn, c0=c0, r0=r0, ev=ev, qt=qt, stp=stp:
                              e.matmul(stp[0:cn, 128:128 + cn], lhsT=qk[r0:r0 + 64, 4 + hp, c0:c0 + cn],
                                       rhs=qt[r0:r0 + 64, 0:cn], start=True, stop=True),
                              reads=[("qk", 4 + hp), ("qt", qi)], writes=[("ps", stb)])
                        pi = nxt("PT")
                        PT = self.PT[pi]
                        T_.dve(lambda e, PT=PT, stp=stp, ev=ev, cn=cn, j=j, hh=hh:
                               e.scalar_tensor_tensor(out=PT[0:cn, 0:cn], in0=stp[0:cn, 128:128 + cn],
                                                      scalar=avec[0:cn, j, hh:hh + 1], in1=maskT[0:cn, 0:cn],
                                                      op0=ALU.mult, op1=ALU.mult),
                               reads=[("ps", stb), ("avec", j), "maskT"], writes=[("PT", pi)])
                        hpb = 6 + ev
                        hps = self.psum[hpb]
                        ho = 0
                        T_.pe(lambda e, PT=PT, cn=cn, j=j, hh=hh, hps=hps, ho=ho:
                              e.matmul(hps[:, ho:ho + cn], lhsT=Vext[0:cn, j, hh, 0:128], rhs=PT[0:cn, 0:cn], start=True, stop=False),
                              reads=[("V", j, hh // 4), ("PT", pi)], writes=[("ps", hpb)])
                        T_.pe(lambda e, cn=cn, hp=hp, r0=r0, qt=qt, hps=hps, ho=ho:
                              e.matmul(hps[:, ho:ho + cn], lhsT=Cbf[r0:r0 + 64, hp, :], rhs=qt[r0:r0 + 64, 0:cn], start=False, stop=True),
                              reads=[("Cbf", hp), ("qt", qi)], writes=[("ps", hpb)])
                        T_.pe(lambda e, PT=PT, cn=cn, hps=hps, ho=ho:
                              e.matmul(hps[:, ho + 128:ho + 128 + cn], lhsT=ones[0:cn, :], rhs=PT[0:cn, 0:cn], start=True, stop=False),
                              reads=["ones", ("PT", pi)], writes=[("ps", hpb)])
                        T_.pe(lambda e, cn=cn, hp=hp, r0=r0, qt=qt, hps=hps, ho=ho:
                              e.matmul(hps[:, ho + 128:ho + 128 + cn], lhsT=nrep[r0:r0 + 64, hp, :], rhs=qt[r0:r0 + 64, 0:cn], start=False, stop=True),
                              reads=[("nrep", hp), ("qt", qi)], writes=[("ps", hpb)])
                        di = nxt("dn")
                        dn = self.dn[di]
                        T_.act(lambda e, dn=dn, hps=hps, ho=ho, cn=cn:
                               e.activation(out=dn[:, 0:cn], in_=hps[:, ho + 128:ho + 128 + cn], func=AF.Abs),
                               reads=[("ps", hpb)], writes=[("dn", di)])
                        T_.dve(lambda e, dn=dn, cn=cn: e.tensor_scalar(dn[:, 0:cn], dn[:, 0:cn], 1.0, None, ALU.max),
                               reads=[("dn", di)], writes=[("dn", di)])
                        T_.dve(lambda e, dn=dn, cn=cn: e.reciprocal(dn[:, 0:cn], dn[:, 0:cn]), reads=[("dn", di)], writes=[("dn", di)])
                        t1i = nxt("t1")
                        t1 = self.t1[t1i]
                        T_.dve(lambda e, t1=t1, dn=dn, hps=hps, ho=ho, cn=cn:
                               e.tensor_tensor(out=t1[:, 0:cn], in0=hps[:, ho:ho + cn], in1=dn[:, 0:cn], op=ALU.mult),
                               reads=[("ps", hpb), ("dn", di)], writes=[("t1", t1i)])
                        T_.dve(lambda e, t1=t1, hh=hh, c0=c0, cn=cn:
                               e.tensor_tensor(out=hsg[:, hh, c0:c0 + cn], in0=t1[:, 0:cn], in1=sgo[:, hh, c0:c0 + cn], op=ALU.mult),
                               reads=[("t1", t1i), ("sgo", hh)], writes=[("hsg", hh)])
                        T_.pe(lambda e, Ka=Ka, cn=cn, j=j, hh=hh, r0=r0, ups=ups:
                              e.matmul(ups[r0:r0 + 64, 0:129], lhsT=Ka[0:cn, r0:r0 + 64], rhs=Vext[0:cn, j, hh, 0:129], start=True, stop=True),
                              reads=[("Ka", kai), ("V", j, hh // 4)], writes=[("ps", 3)])

    def _mlstm_state(self, hp, cn, bexp, bi, ups):
        T_ = self.T
        Cst, Cbf, nrep, Cd = self.Cst, self.Cbf, self.nrep, self.Cd
        if True:
            if True:
                if True:
                    dec = bexp[:, cn - 1:cn]
                    T_.dve(lambda e, hp=hp, dec=dec: e.tensor_scalar(Cd[:, :], Cst[:, hp, :], dec, None, ALU.mult),
                           reads=[("C", hp), ("bexp", bi)], writes=["Cd"])
                    T_.dve(lambda e, hp=hp, dec=dec, ups=ups:
                           e.scalar_tensor_tensor(out=Cst[:, hp, :], in0=ups[:, 0:129], scalar=dec, in1=Cd[:, :], op0=ALU.mult, op1=ALU.add),
                           reads=[("ps", 3), "Cd", ("bexp", bi)], writes=[("C", hp)])
                    T_.act(lambda e, hp=hp: e.activation(out=Cbf[:, hp, :], in_=Cst[:, hp, 0:128], func=AF.Copy),
                           reads=[("C", hp)], writes=[("Cbf", hp)])
                    T_.act(lambda e, hp=hp: e.activation(out=nrep[:, hp, :], in_=Cst[:, hp, 128:129].to_broadcast([128, 128]), func=AF.Copy),
                           reads=[("C", hp)], writes=[("nrep", hp)])

    def _mlstm_views(self, setk):
        if setk == 0:
            EA = self.xin[0][:, 0:1024].rearrange("p (a t) -> p a t", a=8)
            rep = [self.xin[1][:, i * 512:(i + 1) * 512].rearrange("p (h d) -> p h d", h=8) for i in range(2)]
            return EA, rep, self.qt_all, self.kt_all, self.Ka_all, self.mPT
        yt = self.yt
        EA = yt[:, 0:2, :].rearrange("p a (b t) -> p (a b) t", t=128)
        rep = [yt[:, 2 + i, :].rearrange("p (h d) -> p h d", h=8) for i in range(2)]
        qt = yt[:, 4, 0:256].bitcast(BF16).rearrange("p (a t) -> p a t", a=4)
        kt = yt[:, 4, 256:512].bitcast(BF16).rearrange("p (a t) -> p a t", a=4)
        Ka = yt[:, 5, 0:256].bitcast(BF16).rearrange("p (a t) -> p a t", a=4)
        mPT = yt[:, 6, :].bitcast(BF16).rearrange("p (a t) -> p a t", a=8)
        return EA, rep, qt, kt, Ka, mPT

    def _mlstm_set_tokens(self, setk):
        return ([("EA", setk, 0), ("EA", setk, 1), ("rep", setk, 0), ("rep", setk, 1), ("qt_all", setk), ("kt_all", setk),
                 ("Ka_all", setk), ("mPT", setk, 0), ("mPT", setk, 1)])

    def _mlstm_front(self, j, cn, c0, setk):
        T_ = self.T
        qk, Vext, hsg, sgo = self.qk, self.Vext, self.hsg, self.sgo
        ident, identb, maskT, ones = self.ident, self.identb, self.maskT, self.ones_bf
        Cst, Cbf, nrep = self.Cst, self.Cbf, self.nrep
        gsb, lneg, nla = self.gsb, self.lneg, self.nla
        EA, rep, qt_all, kt_all, Ka_all, mPT = self._mlstm_views(setk)
        dns = [self.tmp[0][:, :].rearrange("p (h t) -> p h t", h=4), self.rstd[0][:, :].rearrange("p (h t) -> p h t", h=4)]
        t1s = [self.tmp[1][:, :].rearrange("p (h t) -> p h t", h=4), self.rstd[1][:, :].rearrange("p (h t) -> p h t", h=4)]
        dtoks = [("tmp", 0), ("rstd", 0)]
        ttoks = [("tmp", 1), ("rstd", 1)]
        hbanks = [(6, 7), (0, 1)]
        ps = self.psum
        T_.dve(lambda e: e.tensor_copy(rep[0][0:cn, :, :], lneg[0:cn, j, :].unsqueeze(2).to_broadcast([cn, 8, 64])),
               reads=[("lneg", j)], writes=[("rep", setk, 0)])
        T_.dve(lambda e: e.tensor_copy(rep[1][0:cn, :, :], nla[0:cn, j, :].unsqueeze(2).to_broadcast([cn, 8, 64])),
               reads=[("nla", j)], writes=[("rep", setk, 1)])
        for hp in range(4):
            bank, o = hp // 2, (hp % 2) * 256
            T_.pe(lambda e, hp=hp, bank=bank, o=o:
                  e.matmul(ps[bank][:, o:o + cn], lhsT=rep[0][0:cn, 2 * hp:2 * hp + 2, :].rearrange("p a b -> p (a b)"),
                           rhs=maskT[0:cn, 0:cn], start=True, stop=True),
                  reads=[("rep", setk, 0), "maskT"], writes=[("ps", bank)])
            T_.pe(lambda e, hp=hp, bank=bank, o=o:
                  e.matmul(ps[bank][:, o + 128:o + 128 + cn], lhsT=rep[1][0:cn, 2 * hp:2 * hp + 2, :].rearrange("p a b -> p (a b)"),
                           rhs=ident[0:cn, 0:cn], start=True, stop=True),
                  reads=[("rep", setk, 1), "ident"], writes=[("ps", bank)])
        for b in range(2):
            T_.act(lambda e, b=b: e.activation(out=EA[:, 4 * b:4 * b + 4, 0:cn],
                                               in_=ps[b][:, :].rearrange("p (a t) -> p a t", a=4)[:, :, 0:cn], func=AF.Exp, scale=-1.0),
                   reads=[("ps", b)], writes=[("EA", setk, b)])
        T_.dve(lambda e: e.scalar_tensor_tensor(out=qt_all[:, :, 0:cn], in0=qk[:, 0:4, c0:c0 + cn], scalar=0.125,
                                                in1=EA[:, 0:8:2, 0:cn], op0=ALU.mult, op1=ALU.mult),
               reads=[("qk", c) for c in range(4)] + [("EA", setk, 0), ("EA", setk, 1)], writes=[("qt_all", setk)])
        T_.dve(lambda e: e.tensor_tensor(out=kt_all[:, :, 0:cn], in0=qk[:, 4:8, c0:c0 + cn], in1=EA[:, 1:8:2, 0:cn], op=ALU.mult),
               reads=[("qk", c) for c in range(4, 8)] + [("EA", setk, 0), ("EA", setk, 1)], writes=[("kt_all", setk)])
        for hp in range(4):
            T_.pe(lambda e, hp=hp: e.transpose(self.psum2_bf[0:cn, hp * 128:(hp + 1) * 128], kt_all[:, hp, 0:cn], identb[:]),
                  reads=[("kt_all", setk), "identb"], writes=[("ps", 2)])
        T_.dve(lambda e: e.tensor_copy(Ka_all[0:cn, :, :], self.psum2_bf[0:cn, 0:512].rearrange("p (a d) -> p a d", a=4)),
               reads=[("ps", 2)], writes=[("Ka_all", setk)])
        for hh in range(8):
            hp, r0 = hh // 2, 64 * (hh % 2)
            bank, off = 4 + hh % 2, hp * 128
            T_.pe(lambda e, hp=hp, r0=r0, bank=bank, off=off:
                  e.matmul(ps[bank][0:cn, off:off + cn], lhsT=kt_all[r0:r0 + 64, hp, 0:cn], rhs=qt_all[r0:r0 + 64, hp, 0:cn],
                           start=True, stop=True),
                  reads=[("kt_all", setk), ("qt_all", setk)], writes=[("ps", bank)])
        for a in range(2):
            T_.dve(lambda e, a=a: e.tensor_tensor(out=mPT[0:cn, 4 * a:4 * a + 4, 0:cn],
                                                  in0=ps[4 + a][0:cn, :].rearrange("p (h t) -> p h t", h=4)[:, :, 0:cn],
                                                  in1=maskT[0:cn, 0:cn].unsqueeze(1).to_broadcast([cn, 4, cn]), op=ALU.mult),
                   reads=[("ps", 4 + a), "maskT"], writes=[("mPT", setk, a)])

    def _mlstm_back(self, j, cn, c0, setk):
        T_ = self.T
        qk, Vext, hsg, sgo = self.qk, self.Vext, self.hsg, self.sgo
        ident, identb, maskT, ones = self.ident, self.identb, self.maskT, self.ones_bf
        Cst, Cbf, nrep = self.Cst, self.Cbf, self.nrep
        gsb, lneg, nla = self.gsb, self.lneg, self.nla
        EA, rep, qt_all, kt_all, Ka_all, mPT = self._mlstm_views(setk)
        dns = [self.tmp[0][:, :].rearrange("p (h t) -> p h t", h=4), self.rstd[0][:, :].rearrange("p (h t) -> p h t", h=4)]
        t1s = [self.tmp[1][:, :].rearrange("p (h t) -> p h t", h=4), self.rstd[1][:, :].rearrange("p (h t) -> p h t", h=4)]
        dtoks = [("tmp", 0), ("rstd", 0)]
        ttoks = [("tmp", 1), ("rstd", 1)]
        hbanks = [(6, 7), (0, 1)]
        ps = self.psum
        for a in range(2):
            hb, db = hbanks[a]
            dn, t1, dtok, ttok = dns[a], t1s[a], dtoks[a], ttoks[a]
            for hi in range(4):
                hh = 2 * hi + a
                hp, r0, off = hi, 64 * a, hi * 128
                T_.pe(lambda e, hh=hh, off=off, hi=hi, a=a, hb=hb, db=db: e.matmul(ps[hb][:, off:off + cn], lhsT=Vext[0:cn, j, hh, 0:128], rhs=mPT[0:cn, 4 * a + hi, 0:cn],
                                                         start=True, stop=False),
                      reads=[("V", j, hh // 4), ("mPT", setk, a)], writes=[("ps", hb)])
                T_.pe(lambda e, hp=hp, r0=r0, off=off, hb=hb, db=db: e.matmul(ps[hb][:, off:off + cn], lhsT=Cbf[r0:r0 + 64, hp, :], rhs=qt_all[r0:r0 + 64, hp, 0:cn],
                                                                start=False, stop=True),
                      reads=["Cbfall", ("qt_all", setk)], writes=[("ps", hb)])
                T_.pe(lambda e, hh=hh, off=off, hi=hi, a=a, hb=hb, db=db: e.matmul(ps[db][:, off:off + cn], lhsT=ones[0:cn, :], rhs=mPT[0:cn, 4 * a + hi, 0:cn],
                                                         start=True, stop=False),
                      reads=["ones", ("mPT", setk, a)], writes=[("ps", db)])
                T_.pe(lambda e, hp=hp, r0=r0, off=off, hb=hb, db=db: e.matmul(ps[db][:, off:off + cn], lhsT=nrep[r0:r0 + 64, hp, :], rhs=qt_all[r0:r0 + 64, hp, 0:cn],
                                                                start=False, stop=True),
                      reads=["nrepall", ("qt_all", setk)], writes=[("ps", db)])
            T_.act(lambda e, dn=dn, db=db: e.activation(out=dn[:, :, 0:cn], in_=ps[db][:, :].rearrange("p (h t) -> p h t", h=4)[:, :, 0:cn], func=AF.Abs),
                   reads=[("ps", db)], writes=[dtok])
            T_.dve(lambda e, dn=dn: e.tensor_scalar(dn[:, :, 0:cn], dn[:, :, 0:cn], 1.0, None, ALU.max), reads=[dtok], writes=[dtok])
            T_.dve(lambda e, dn=dn: e.reciprocal(dn[:, :, 0:cn], dn[:, :, 0:cn]), reads=[dtok], writes=[dtok])
            T_.dve(lambda e, dn=dn, t1=t1, hb=hb: e.tensor_tensor(out=t1[:, :, 0:cn], in0=ps[hb][:, :].rearrange("p (h t) -> p h t", h=4)[:, :, 0:cn],
                                             in1=dn[:, :, 0:cn], op=ALU.mult),
                   reads=[("ps", hb), dtok], writes=[ttok])
            T_.dve(lambda e, a=a, t1=t1: e.tensor_tensor(out=hsg[:, a:8:2, c0:c0 + cn], in0=t1[:, :, 0:cn],
                                                  in1=sgo[:, a:8:2, c0:c0 + cn], op=ALU.mult),
                   reads=[ttok] + [("sgo", c) for c in range(a, 8, 2)],
                   writes=[("hsg", c) for c in range(a, 8, 2)])
        for hh in range(8):
            hp, r0 = hh // 2, 64 * (hh % 2)
            bank, coff = 4 + hp // 2, (hp % 2) * 256
            T_.pe(lambda e, hh=hh, hp=hp, r0=r0, bank=bank, coff=coff:
                  e.matmul(ps[bank][r0:r0 + 64, coff:coff + 129], lhsT=Ka_all[0:cn, hp, r0:r0 + 64], rhs=Vext[0:cn, j, hh, 0:129],
                           start=True, stop=True),
                  reads=[("Ka_all", setk), ("V", j, hh // 4)], writes=[("ps", bank)])
        for b in range(2):
            T_.dve(lambda e, b=b: e.tensor_tensor(out=Cst[:, 2 * b:2 * b + 2, :], in0=ps[4 + b][:, 0:512].rearrange("p (a v) -> p a v", a=2)[:, :, 0:129],
                                                  in1=Cst[:, 2 * b:2 * b + 2, :], op=ALU.add),
                   reads=[("ps", 4 + b), "Call"], writes=["Call"])
        T_.dve(lambda e: e.tensor_tensor(out=Cst[:, :, :], in0=Cst[:, :, :], in1=EA[:, 0:8:2, cn - 1:cn].to_broadcast([128, 4, 129]), op=ALU.mult),
               reads=["Call", ("EA", setk, 0), ("EA", setk, 1)], writes=["Call"])
        T_.act(lambda e: e.activation(out=Cbf[:, :, :], in_=Cst[:, :, 0:128], func=AF.Copy), reads=["Call"], writes=["Cbfall"])
        T_.act(lambda e: e.activation(out=nrep[:, :, :], in_=Cst[:, :, 128:129].to_broadcast([128, 4, 128]), func=AF.Copy),
               reads=["Call"], writes=["nrepall"])

    def _mlstm_outproj(self, ti, n):
        T_ = self.T
        w_out = self.a_w_out
        sq, yt, hsg = self.sq, self.yt, self.hsg
        if True:
            for dh in range(2):
                slab, stok = self.wload(w_out[:, dh * 512:(dh + 1) * 512], 8, 512)
                for di in range(4):
                    d = dh * 4 + di
                    yb = di
                    yps = self.psum[yb]
                    for k in range(8):
                        T_.pe(lambda e, k=k, di=di, slab=slab, yps=yps:
                              e.matmul(yps[:, 0:n], lhsT=slab[:, k, di * 128:(di + 1) * 128], rhs=hsg[:, k, 0:n],
                                       start=(k == 0), stop=(k == 7)),
                              reads=[stok, ("hsg", k)], writes=[("ps", yb)])
                    T_.act(lambda e, d=d, yps=yps: e.activation(out=sq[:, d, 0:n], in_=yps[:, 0:n], func=AF.Square),
                           reads=[("ps", yb)], writes=[("sq", d)])
                    T_.act(lambda e, d=d, yps=yps: e.activation(out=yt[:, d, 0:n], in_=yps[:, 0:n], func=AF.Copy,
                                                                scale=self.gain(0, 3, d)),
                           reads=[("ps", yb), "pv"], writes=[("yt", d)])
            self.postnorm_residual2(ti, psb=5, first=True)

    def hgrn2_stage(self):
        T_ = self.T
        pv, lbv, oml = self.pv, self.lbv, self.oml
        self.arena_handoff([("Vb", jb) for jb in range(4)] + [("hsgb", c) for c in range(8)] + [("X", i) for i in range(10)])
        self.arena2_handoff(["Sall", "Sbfall", ("PTa", 0, 0), ("PTa", 0, 1), ("kTa", 0)] + [("decs", c) for c in range(8)])
        x0 = self.xin[0]
        self.PTa_s = [self.PTa, x0[:, 0:512].bitcast(BF16).rearrange("p (h t) -> p h t", h=8)]
        self.kTa_s = [self.kTa, x0[:, 512:1024].bitcast(BF16).rearrange("p (h t) -> p h t", h=8)]
        T_.alias([("PTa", 1, 0), ("PTa", 1, 1), ("kTa", 1)], [("xin", 0)])
        T_.dve(lambda e: e.tensor_tensor(out=lbv[:], in0=pv[:, 152:160], in1=pv[:, 144:152], op=ALU.subtract),
               reads=["pv"], writes=["lbv"])
        T_.act(lambda e: e.activation(out=lbv[:], in_=lbv[:], func=AF.Sigmoid), reads=["lbv"], writes=["lbv"])
        T_.dve(lambda e: e.tensor_scalar(oml[:], lbv[:], -1.0, 1.0, ALU.mult, ALU.add), reads=["lbv"], writes=["oml"])
        T_.dve(lambda e: e.memset(self.Sst[:], 0.0), writes=["Sall"])
        T_.dve(lambda e: e.memset(self.Sbf[:], 0.0), writes=["Sbfall"])
        rot = {"pp": 0, "pv": 0, "ql": 0, "kl": 0, "kT": 0, "dec": 0, "PT": 0}

        def nxt(k, m=2):
            v = rot[k]
            rot[k] = (v + 1) % m
            return v

        for ti, (t0, n) in enumerate(self.TILES):
            self._hg_tile(ti, t0, n, nxt)
        T_.alias([("xin", 0)], [("PTa", 1, 0), ("PTa", 1, 1), ("kTa", 1)])

    def _hg_tile(self, ti, t0, n, nxt):
        T_ = self.T
        w_in = self.b_w_in
        Vb, sgo = self.Vb, self.sgo
        ub, ui = self.prenorm(1, 2, ti, psb=5)
        utoks = [("u", ui, k) for k in range(8)]
        nblk = (n + 127) // 128
        def vg_gen():
            for half in range(2):
                slab, stok = self.wload(w_in[:, 2048 + half * 512: 2048 + (half + 1) * 512], 8, 512, slot=(0, 3)[half])
                for jb in range(nblk):
                    bn = min(128, n - jb * 128)
                    pb = 4 + (jb % 2)
                    pst = self.psum[pb]
                    for k in range(8):
                        T_.pe(lambda e, k=k, jb=jb, bn=bn, slab=slab, pst=pst:
                              e.matmul(pst[0:bn, 0:512], lhsT=ub[:, k, jb * 128:jb * 128 + bn], rhs=slab[:, k, 0:512],
                                       start=(k == 0), stop=(k == 7)),
                              reads=[stok, utoks[k]], writes=[("ps", pb)])
                    T_.act(lambda e, jb=jb, bn=bn, half=half, pst=pst:
                           e.activation(out=Vb[0:bn, jb, half * 512:(half + 1) * 512], in_=pst[0:bn, :], func=AF.Copy),
                           reads=[("ps", pb)], writes=[("Vb", jb)])
                    yield
            for half in range(2):
                slab, stok = self.wload(w_in[:, 3072 + half * 512: 3072 + (half + 1) * 512], 8, 512, slot=(0, 3)[half])
                for cc in range(4):
                    c8 = half * 4 + cc
                    pb = 6 + (cc % 2)
                    pst = self.psum[pb]
                    for k in range(8):
                        T_.pe(lambda e, k=k, cc=cc, slab=slab, pst=pst:
                              e.matmul(pst[:, 0:n], lhsT=slab[:, k, cc * 128:(cc + 1) * 128], rhs=ub[:, k, 0:n],
                                       start=(k == 0), stop=(k == 7)),
                              reads=[stok, utoks[k]], writes=[("ps", pb)])
                    T_.act(lambda e, pst=pst, c8=c8: e.activation(out=sgo[:, c8, 0:n], in_=pst[:, 0:n], func=AF.Sigmoid),
                           reads=[("ps", pb)], writes=[("sgo", c8)])
                    yield
        slabs = {}

        def get_slabs(hg):
            if hg not in slabs:
                q_ = self.wload(w_in[:, hg * 512:(hg + 1) * 512], 8, 512, slot=1)
                f_ = self.wload(w_in[:, 1024 + hg * 512: 1024 + (hg + 1) * 512], 8, 512, slot=2)
                slabs[hg] = (q_, f_)
            return slabs[hg]

        kui = (1 - ui) if ui in (0, 1) else 0
        shared = {"QT": self.sq, "KT": self.ubuf[kui], "kui": kui}

        def front(c):
            (qslab, qtok), (fslab, ftok) = get_slabs(c // 4)
            return self._hg_front(n, ub, utoks, c, c % 4, qslab, qtok, fslab, ftok, nxt, c % 2, shared)

        def drive(gens):
            gens = [g for g in gens if g is not None]
            while gens:
                for g in list(gens):
                    try:
                        next(g)
                    except StopIteration:
                        gens.remove(g)

        vg = vg_gen()
        for c in range(0, 8, 2):
            drive([front(c), front(c + 1)] + ([vg] if c < 4 else []))
        drive([vg])
        self.wslot_next = 0
        self._hg_block_front(0, min(128, n), kui, 0)
        for jb in range(nblk):
            if jb + 1 < nblk:
                self._hg_block_front(jb + 1, min(128, n - (jb + 1) * 128), kui, (jb + 1) % 2)
            self._hg_block_b(jb, min(128, n - jb * 128), kui, jb % 2)
        self._hg_out(ti, n)

    def _hg_front(self, n, ub, utoks, c, cc, qslab, qtok, fslab, ftok, nxt, setk, res):
        T_ = self.T
        pv = self.pv
        X0, X1, X2, X3, X4 = self.X[5 * setk:5 * setk + 5]
        xo = 5 * setk
        qb, fb = 2 * setk, 2 * setk + 1
        qps, fps = self.psum[qb], self.psum[fb]
        for k in range(8):
            T_.pe(lambda e, k=k: e.matmul(qps[:, 0:n], lhsT=qslab[:, k, cc * 128:(cc + 1) * 128], rhs=ub[:, k, 0:n],
                                          start=(k == 0), stop=(k == 7)),
                  reads=[qtok, utoks[k]], writes=[("ps", qb)])
        yield
        T_.act(lambda e: e.activation(out=X0[:, 0:n], in_=qps[:, 0:n], func=AF.Silu), reads=[("ps", qb)], writes=[("X", xo + 0)])
        yield
        for k in range(8):
            T_.pe(lambda e, k=k: e.matmul(fps[:, 0:n], lhsT=fslab[:, k, cc * 128:(cc + 1) * 128], rhs=ub[:, k, 0:n],
                                          start=(k == 0), stop=(k == 7)),
                  reads=[ftok, utoks[k]], writes=[("ps", fb)])
        yield
        T_.act(lambda e: e.activation(out=X1[:, 0:n], in_=fps[:, 0:n], func=AF.Sigmoid, bias=pv[:, 136 + c:137 + c]),
               reads=[("ps", fb), "pv"], writes=[("X", xo + 1)])
        yield
        T_.dve(lambda e: e.tensor_scalar(X1[:, 0:n], X1[:, 0:n], self.oml[:, c:c + 1], self.lbv[:, c:c + 1], ALU.mult, ALU.add),
               reads=[("X", xo + 1), "oml", "lbv"], writes=[("X", xo + 1)])
        yield
        T_.act(lambda e: e.activation(out=X2[:, 0:n], in_=X1[:, 0:n], func=AF.Ln), reads=[("X", xo + 1)], writes=[("X", xo + 2)])
        yield
        T_.dve(lambda e: e.tensor_scalar(X1[:, 0:n], X1[:, 0:n], -1.0, 1.0, ALU.mult, ALU.add),
               reads=[("X", xo + 1), ("X", xo + 2)], writes=[("X", xo + 1)])
        yield
        nch = (n + 63) // 64
        for jc in range(nch):
            if jc == 4:
                yield
            c0 = jc * 64
            cl = min(64, n - c0)
            T_.dve(lambda e, c0=c0, cl=cl: e.tensor_tensor_scan(out=X3[:, c0:c0 + cl], data0=self.ones_f[:, 0:cl],
                                                                 data1=X2[:, c0:c0 + cl], initial=0.0, op0=ALU.mult, op1=ALU.add),
                   reads=[("X", xo + 2), "ones_f"], writes=[("X", xo + 3)])
        yield
        T_.act(lambda e: e.activation(out=X4[:, 0:n], in_=X3[:, 0:n], func=AF.Exp), reads=[("X", xo + 3)], writes=[("X", xo + 4)])
        T_.act(lambda e: e.activation(out=X2[:, 0:n], in_=X3[:, 0:n], func=AF.Exp, scale=-1.0), reads=[("X", xo + 3)], writes=[("X", xo + 2)])
        yield
        QT, KT, kui = res["QT"], res["KT"], res["kui"]
        decs = self.decs_all
        T_.dve(lambda e: e.tensor_tensor(out=QT[:, c, 0:n], in0=X0[:, 0:n], in1=X4[:, 0:n], op=ALU.mult),
               reads=[("X", xo + 0), ("X", xo + 4)], writes=[("sq", c)])
        T_.dve(lambda e: e.tensor_tensor(out=KT[:, c, 0:n], in0=X1[:, 0:n], in1=X2[:, 0:n], op=ALU.mult),
               reads=[("X", xo + 1), ("X", xo + 2)], writes=[("u", kui, c)])
        if n % 64 == 0:
            T_.act(lambda e: e.activation(out=decs[:, c, 0:nch], in_=X4[:, 63:n:64], func=AF.Copy), reads=[("X", xo + 4)], writes=[("decs", c)])
        else:
            assert nch == 1
            T_.act(lambda e: e.activation(out=decs[:, c, 0:1], in_=X4[:, n - 1:n], func=AF.Copy), reads=[("X", xo + 4)], writes=[("decs", c)])
        yield

    def _hg_blocks(self, n, c, r, nxt):
        nblk = (n + 127) // 128
        for jb in range(nblk):
            yield from self._hg_block(c, jb, min(128, n - jb * 128), r["qtl"], r["qi"], r["ktl"], r["ki"], r["decs"], r["di"], nxt)


    def _hg_block(self, c, jb, bn, qtl, qi, ktl, ki, decs, di, nxt):
        T_ = self.T
        Vb, sgo, yt = self.Vb, self.sgo, self.yt
        Sst, Sbf, Sd = self.Sst, self.Sbf, self.Sd
        b0 = jb * 128
        aps, ops, sups = self.psum[4], self.psum[5], self.psum[7]
        T_.pe(lambda e: e.matmul(aps[0:bn, 0:bn], lhsT=ktl[:, b0:b0 + bn], rhs=qtl[:, b0:b0 + bn], start=True, stop=True),
              reads=[("ktl", ki), ("qtl", qi)], writes=[("ps", 4)])
        yield
        pi = nxt("PT")
        PT = self.PT[pi]
        T_.dve(lambda e: e.tensor_tensor(out=PT[0:bn, 0:bn], in0=aps[0:bn, 0:bn], in1=self.maskBD[0:bn, 0:bn], op=ALU.mult),
               reads=[("ps", 4), "maskBD"], writes=[("PT", pi)])
        yield
        T_.pe(lambda e: e.matmul(ops[:, 0:bn], lhsT=Vb[0:bn, jb, c * 128:(c + 1) * 128], rhs=PT[0:bn, 0:bn], start=True, stop=False),
              reads=[("Vb", jb), ("PT", pi)], writes=[("ps", 5)])
        kps = self.psum6_bf[:, 0:128]
        T_.pe(lambda e: e.transpose(kps[0:bn, :], ktl[:, b0:b0 + bn], self.identb[:]),
              reads=[("ktl", ki), "identb"], writes=[("ps", 6)])
        yield
        kti = nxt("kT")
        kTs = self.kTs[kti]
        T_.act(lambda e: e.activation(out=kTs[0:bn, :], in_=kps[0:bn, :], func=AF.Copy), reads=[("ps", 6)], writes=[("kTs", kti)])
        chunks = [(0, min(64, bn))] + ([(64, bn - 64)] if bn > 64 else [])
        for e_i, (r0, cl) in enumerate(chunks):
            yield
            yield from self._hg_chunk(c, jb, b0, r0, cl, e_i == len(chunks) - 1, qtl, qi, kTs, kti, decs, di, 2 * jb + e_i)
        yield
        T_.dve(lambda e: e.tensor_tensor(out=yt[:, c, b0:b0 + bn], in0=ops[:, 0:bn], in1=sgo[:, c, b0:b0 + bn], op=ALU.mult),
               reads=[("ps", 5), ("sgo", c)], writes=[("yt", c)])

    def _hg_chunk(self, c, jb, b0, r0, cl, last, qtl, qi, kTs, kti, decs, di, jc):
        T_ = self.T
        Vb = self.Vb
        Sst, Sbf, Sd = self.Sst, self.Sbf, self.Sd
        ops, sups = self.psum[5], self.psum[7]
        T_.pe(lambda e: e.matmul(ops[:, r0:r0 + cl], lhsT=Sbf[:, c, :], rhs=qtl[:, b0 + r0:b0 + r0 + cl], start=False, stop=last),
              reads=[("Sbf", c), ("qtl", qi)], writes=[("ps", 5)])
        T_.pe(lambda e: e.matmul(sups[:, 0:128], lhsT=kTs[r0:r0 + cl, :], rhs=Vb[r0:r0 + cl, jb, c * 128:(c + 1) * 128], start=True, stop=True),
              reads=[("kTs", kti), ("Vb", jb)], writes=[("ps", 7)])
        yield
        dec = decs[:, jc:jc + 1]
        T_.dve(lambda e: e.tensor_scalar(Sd[:, :], Sst[:, c, :], dec, None, ALU.mult),
               reads=[("S", c), ("decs", di)], writes=["Sd"])
        T_.dve(lambda e: e.scalar_tensor_tensor(out=Sst[:, c, :], in0=sups[:, 0:128], scalar=dec, in1=Sd[:, :], op0=ALU.mult, op1=ALU.add),
               reads=[("ps", 7), "Sd", ("decs", di)], writes=[("S", c)])
        yield
        T_.act(lambda e: e.activation(out=Sbf[:, c, :], in_=Sst[:, c, :], func=AF.Copy), reads=[("S", c)], writes=[("Sbf", c)])

    def _hg_block_front(self, jb, bn, kui, setk):
        T_ = self.T
        QT, KT = self.sq, self.ubuf[kui]
        Vb, sgo, yt = self.Vb, self.sgo, self.yt
        PTa, kTa = self.PTa_s[setk], self.kTa_s[setk]
        b0 = jb * 128
        for hh in range(8):
            bank, off = 4 + hh // 4, (hh % 4) * 128
            T_.pe(lambda e, hh=hh, bank=bank, off=off:
                  e.matmul(self.psum[bank][0:bn, off:off + bn], lhsT=KT[:, hh, b0:b0 + bn], rhs=QT[:, hh, b0:b0 + bn],
                           start=True, stop=True),
                  reads=[("u", kui, hh), ("sq", hh)], writes=[("ps", bank)])
        for a in range(2):
            T_.dve(lambda e, a=a:
                   e.tensor_tensor(out=PTa[0:bn, 4 * a:4 * a + 4, 0:bn],
                                   in0=self.psum[4 + a][0:bn, :].rearrange("p (h t) -> p h t", h=4)[:, :, 0:bn],
                                   in1=self.maskBD[0:bn, 0:bn].unsqueeze(1).to_broadcast([bn, 4, bn]), op=ALU.mult),
                   reads=[("ps", 4 + a), "maskBD"], writes=[("PTa", setk, a)])
        for hh in range(8):
            T_.pe(lambda e, hh=hh: e.transpose(self.psum6_bf[0:bn, hh * 128:(hh + 1) * 128], KT[:, hh, b0:b0 + bn], self.identb[:]),
                  reads=[("u", kui, hh), "identb"], writes=[("ps", 6)])
        T_.act(lambda e: e.activation(out=kTa[0:bn, :, :], in_=self.psum6_bf[0:bn, 0:1024].rearrange("p (h d) -> p h d", h=8), func=AF.Copy),
               reads=[("ps", 6)], writes=[("kTa", setk)])

    def _hg_block_b(self, jb, bn, kui, setk):
        T_ = self.T
        QT, KT = self.sq, self.ubuf[kui]
        Vb, sgo, yt = self.Vb, self.sgo, self.yt
        Sst, Sbf, decs = self.Sst, self.Sbf, self.decs_all
        PTa, kTa = self.PTa_s[setk], self.kTa_s[setk]
        b0 = jb * 128
        chunks = [(0, min(64, bn))] + ([(64, bn - 64)] if bn > 64 else [])
        for e_i, (r0, cl) in enumerate(chunks):
            jc = 2 * jb + e_i
            for hh in range(8):
                bank, off = hh // 4, (hh % 4) * 128
                T_.pe(lambda e, hh=hh, bank=bank, off=off, r0=r0, cl=cl:
                      e.matmul(self.psum[bank][:, off + r0:off + r0 + cl], lhsT=Vb[r0:r0 + cl, jb, hh * 128:(hh + 1) * 128],
                               rhs=PTa[r0:r0 + cl, hh, r0:r0 + cl], start=True, stop=False),
                      reads=[("Vb", jb), ("PTa", setk, hh // 4)], writes=[("ps", bank)])
                T_.pe(lambda e, hh=hh, bank=bank, off=off, r0=r0, cl=cl:
                      e.matmul(self.psum[bank][:, off + r0:off + r0 + cl], lhsT=Sbf[:, hh, :],
                               rhs=QT[:, hh, b0 + r0:b0 + r0 + cl], start=False, stop=True),
                      reads=["Sbfall", ("sq", hh)], writes=[("ps", bank)])
            for hh in range(8):
                bank, off = 2 + hh // 4, (hh % 4) * 128
                T_.pe(lambda e, hh=hh, bank=bank, off=off, r0=r0, cl=cl:
                      e.matmul(self.psum[bank][:, off:off + 128], lhsT=kTa[r0:r0 + cl, hh, :],
                               rhs=Vb[r0:r0 + cl, jb, hh * 128:(hh + 1) * 128], start=True, stop=True),
                      reads=[("kTa", setk), ("Vb", jb)], writes=[("ps", bank)])
            for a in range(2):
                T_.dve(lambda e, a=a:
                       e.tensor_tensor(out=Sst[:, 4 * a:4 * a + 4, :], in0=self.psum[2 + a][:, :].rearrange("p (h v) -> p h v", h=4),
                                       in1=Sst[:, 4 * a:4 * a + 4, :], op=ALU.add),
                       reads=[("ps", 2 + a), "Sall"], writes=["Sall"])
            T_.dve(lambda e, jc=jc:
                   e.tensor_tensor(out=Sst[:, :, :], in0=Sst[:, :, :],
                                   in1=decs[:, :, jc:jc + 1].to_broadcast([128, 8, 128]), op=ALU.mult),
                   reads=["Sall"] + [("decs", c) for c in range(8)], writes=["Sall"])
            T_.act(lambda e: e.activation(out=Sbf[:, :, :], in_=Sst[:, :, :], func=AF.Copy), reads=["Sall"], writes=["Sbfall"])
        for a in range(2):
            T_.dve(lambda e, a=a:
                   e.tensor_tensor(out=yt[:, 4 * a:4 * a + 4, b0:b0 + bn],
                                   in0=self.psum[a][:, :].rearrange("p (h t) -> p h t", h=4)[:, :, 0:bn],
                                   in1=sgo[:, 4 * a:4 * a + 4, b0:b0 + bn], op=ALU.mult),
                   reads=[("ps", a)] + [("sgo", c) for c in range(4 * a, 4 * a + 4)],
                   writes=[("yt", c) for c in range(4 * a, 4 * a + 4)])

    def _hg_out(self, ti, n):
        T_ = self.T
        w_out = self.b_w_out
        sq, yt, hsgb, pv = self.sq, self.yt, self.hsgb, self.pv
        for c in range(NCH):
            T_.act(lambda e, c=c: e.activation(out=sq[:, c, 0:n], in_=yt[:, c, 0:n], func=AF.Square),
                   reads=[("yt", c)], writes=[("sq", c)])
        rs, rtok = self.rstd_from_sq([("sq", c) for c in range(NCH)], n, 5)
        for c in range(NCH):
            T_.dve(lambda e, c=c: e.scalar_tensor_tensor(out=hsgb[:, c, 0:n], in0=yt[:, c, 0:n], scalar=pv[:, 160 + c:161 + c],
                                                         in1=rs[:, 0:n], op0=ALU.mult, op1=ALU.mult),
                   reads=[("yt", c), rtok, "pv"], writes=[("hsgb", c), ("X", c // 2)])
        for dh in range(2):
            slab, stok = self.wload(w_out[:, dh * 512:(dh + 1) * 512], 8, 512)
            for di in range(4):
                d = dh * 4 + di
                yb = di
                yps = self.psum[yb]
                for k in range(8):
                    T_.pe(lambda e, k=k, di=di, slab=slab, yps=yps:
                          e.matmul(yps[:, 0:n], lhsT=slab[:, k, di * 128:(di + 1) * 128], rhs=hsgb[:, k, 0:n],
                                   start=(k == 0), stop=(k == 7)),
                          reads=[stok, ("hsgb", k), ("X", k // 2)], writes=[("ps", yb)])
                T_.act(lambda e, d=d, yps=yps: e.activation(out=sq[:, d, 0:n], in_=yps[:, 0:n], func=AF.Square),
                       reads=[("ps", yb)], writes=[("sq", d)])
                T_.act(lambda e, d=d, yps=yps: e.activation(out=yt[:, d, 0:n], in_=yps[:, 0:n], func=AF.Copy,
                                                            scale=self.gain(1, 3, d)),
                       reads=[("ps", yb), "pv"], writes=[("yt", d)])
        self.postnorm_residual2(ti, psb=5, first=True)


def pack_pvec(norm_gains, a_conv_w, a_conv_b, b_f_bias, b_lb_raw, b_g_norm):
    pv = np.zeros((256, 128), np.float32)
    pv[0:96] = np.asarray(norm_gains, np.float32).reshape(2 * 6 * 8, 128)
    pv[96:128] = np.asarray(a_conv_w, np.float32).reshape(4 * 8, 128)
    pv[128:136] = np.asarray(a_conv_b, np.float32).reshape(8, 128)
    pv[136:144] = np.asarray(b_f_bias, np.float32).reshape(8, 128)
    pv[144:160] = np.asarray(b_lb_raw, np.float32).reshape(16, 128)
    pv[160:168] = np.asarray(b_g_norm, np.float32).reshape(8, 128)
    return pv


def make_in_maps(inputs, n_cores, nseq):
    x = np.ascontiguousarray(np.asarray(inputs["x"], np.float32))
    pv = pack_pvec(inputs["norm_gains"], inputs["a_conv_w"], inputs["a_conv_b"], inputs["b_f_bias"],
                   inputs["b_lb_raw"], inputs["b_g_norm"])
    common = {
        "meta": np.ascontiguousarray(np.asarray(inputs["meta_tokens"], np.float32)),
        "pvec": pv,
        "gateb": np.ascontiguousarray(np.asarray(inputs["a_gate_b"], np.float32).reshape(1, 16)),
        "ffn_w_in": np.ascontiguousarray(np.asarray(inputs["ffn_w_in"], np.float32)),
        "ffn_w_out": np.ascontiguousarray(np.asarray(inputs["ffn_w_out"], np.float32)),
        "a_w_in": np.ascontiguousarray(np.asarray(inputs["a_w_in"], np.float32)[0]),
        "a_w_out": np.ascontiguousarray(np.asarray(inputs["a_w_out"], np.float32)[0]),
        "b_w_in": np.ascontiguousarray(np.asarray(inputs["b_w_in"], np.float32)[0]),
        "b_w_out": np.ascontiguousarray(np.asarray(inputs["b_w_out"], np.float32)[0]),
    }
    maps = []
    for i in range(n_cores):
        m = dict(common)
        m["x"] = x[i * nseq:(i + 1) * nseq]
        maps.append(m)
    return maps


def kernel(**inputs):
    n_cores = 8
    nseq = 2
    b = Builder(nseq=nseq)
    nc = b.build()
    in_maps = make_in_maps(inputs, n_cores, nseq)
    res = run_bass_kernel_spmd(nc, in_maps, core_ids=list(range(n_cores)))
    outs = [np.asarray(r["out"]) for r in res.results]
    return np.concatenate(outs, axis=0).astype(np.float32)
```

```python
import contextlib
import numpy as np
import concourse.bass as bass
import concourse.mybir as mybir
from concourse.bass_utils import run_bass_kernel_spmd

F32 = mybir.dt.float32
BF16 = mybir.dt.bfloat16
AF = mybir.ActivationFunctionType
ALU = mybir.AluOpType

D = 1024
NCH = 8
SEQ = 2048
NMETA = 16
T = SEQ + NMETA
DFF = 2816
NF = DFF // 128
EPS = 1e-6
A_COLS = 3088
B_COLS = 4096

MAXV = 4000


class Instr:
    __slots__ = ("eng", "fn", "deps", "is_dma", "key", "needs_inc", "sem", "val", "ordn", "idx")

    def __init__(self, eng, fn, is_dma, key):
        self.eng = eng
        self.fn = fn
        self.deps = []
        self.is_dma = is_dma
        self.key = key
        self.needs_inc = is_dma
        self.sem = None
        self.val = None
        self.ordn = None
        self.idx = None


class Tracker:
    ENGS = ("pe", "act", "dve", "pool", "sp")

    def __init__(self, nc, stack):
        self.nc = nc
        self.stack = stack
        self.q = {e: [] for e in self.ENGS}
        self.lastw = {}
        self.readers = {}
        self.nsem = 0

    def _newsem(self, name):
        self.nsem += 1
        return self.stack.enter_context(self.nc.semaphore(f"s{self.nsem}_{name}"))

    def add(self, eng, fn, reads=(), writes=(), dma_key=None):
        ins = Instr(eng, fn, dma_key is not None, dma_key)
        ins.idx = len(self.q[eng])
        deps = []
        for r in reads:
            for w in self.lastw.get(r, ()):
                deps.append((w, "raw"))
        for wr in writes:
            for w in self.lastw.get(wr, ()):
                deps.append((w, "waw"))
            for rd in self.readers.get(wr, ()):
                deps.append((rd, "war"))
        seen = set()
        for d, kind in deps:
            if d is ins or id(d) in seen:
                continue
            if (not d.is_dma) and (not ins.is_dma) and d.eng == eng:
                if eng == "pe":
                    continue
            seen.add(id(d))
            ins.deps.append(d)
            d.needs_inc = True
        for r in reads:
            self.readers.setdefault(r, []).append(ins)
        for wr in writes:
            self.lastw[wr] = [ins]
            self.readers[wr] = []
        self.q[eng].append(ins)
        return ins

    def alias(self, new_tokens, old_tokens):
        oldw, oldr = [], []
        for o in old_tokens:
            oldw.extend(self.lastw.get(o, ()))
            oldr.extend(self.readers.get(o, ()))
        for n in new_tokens:
            self.lastw[n] = list(oldw)
            self.readers[n] = list(oldr)

    def pe(self, fn, reads=(), writes=()):
        return self.add("pe", fn, reads, writes)

    def act(self, fn, reads=(), writes=()):
        return self.add("act", fn, reads, writes)

    def dve(self, fn, reads=(), writes=()):
        return self.add("dve", fn, reads, writes)

    def pool(self, fn, reads=(), writes=()):
        return self.add("pool", fn, reads, writes)

    def dma(self, eng, fn, key, reads=(), writes=()):
        return self.add(eng, fn, reads, writes, dma_key=key)

    def finalize_and_emit(self):
        nc = self.nc
        for e in self.ENGS:
            cnt = 0
            ordn = 0
            sem = None
            for ins in self.q[e]:
                if ins.is_dma or not ins.needs_inc:
                    continue
                if sem is None or cnt >= MAXV:
                    sem = self._newsem(e)
                    cnt = 0
                cnt += 1
                ordn += 1
                ins.sem, ins.val, ins.ordn = sem, cnt, ordn
        dstate = {}
        for e in self.ENGS:
            pass
        for ins in self._all_dma_in_order():
            st = dstate.get(ins.key)
            if st is None or st[1] + 16 > MAXV * 4:
                st = [self._newsem("d"), 0]
                dstate[ins.key] = st
            st[1] += 16
            ins.sem, ins.val = st[0], st[1]

        handles = {"pe": None, "act": None, "dve": None, "pool": None, "sp": None}
        tracker = self

        def emit_queue(e, h):
            waited_ord = {}
            waited_sem = {}
            for ins in tracker.q[e]:
                for d in ins.deps:
                    if d.is_dma:
                        k = id(d.sem)
                        if waited_sem.get(k, 0) >= d.val:
                            continue
                        waited_sem[k] = d.val
                        h.wait_ge(d.sem, d.val)
                    else:
                        if waited_ord.get(d.eng, 0) >= d.ordn:
                            continue
                        waited_ord[d.eng] = d.ordn
                        h.wait_ge(d.sem, d.val)
                bi = ins.fn(h)
                if ins.needs_inc:
                    bi.then_inc(ins.sem, 16 if ins.is_dma else 1)

        with nc.Block() as block:
            @block.tensor
            def _(h):
                emit_queue("pe", h)

            @block.scalar
            def _(h):
                emit_queue("act", h)

            @block.vector
            def _(h):
                emit_queue("dve", h)

            @block.gpsimd
            def _(h):
                emit_queue("pool", h)

            @block.sync
            def _(h):
                emit_queue("sp", h)

    def _all_dma_in_order(self):
        return self._dma_list

    _dma_list = None


def _patch_tracker():
    orig_add = Tracker.add

    def add(self, eng, fn, reads=(), writes=(), dma_key=None):
        ins = orig_add(self, eng, fn, reads, writes, dma_key)
        if dma_key is not None:
            if self._dma_list is None:
                self._dma_list = []
            self._dma_list.append(ins)
        return ins

    Tracker.add = add


_patch_tracker()


class Builder:
    def __init__(self, nseq=2, stages=("ffn_a0", "mlstm", "ffn_b0", "ffn_a1", "hgrn2", "ffn_b1"), dbg=False):
        self.nseq = nseq
        self.stages = stages
        self.nc = bass.Bass("TRN2", target_bir_lowering=False)
        self.stack = contextlib.ExitStack()
        self.T = Tracker(self.nc, self.stack)
        self._uid = 0

    def sb(self, name, shape, dt):
        return self.stack.enter_context(self.nc.sbuf_tensor(name, list(shape), dt))

    def ps(self, name, shape, dt=F32):
        return self.stack.enter_context(self.nc.psum_tensor(name, list(shape), dt))

    def build(self):
        nc = self.nc
        nseq = self.nseq
        self.x = nc.dram_tensor("x", [nseq, SEQ, D], F32, kind="ExternalInput").ap()
        self.meta = nc.dram_tensor("meta", [NMETA, D], F32, kind="ExternalInput").ap()
        self.pvec = nc.dram_tensor("pvec", [256, 128], F32, kind="ExternalInput").ap()
        self.gateb = nc.dram_tensor("gateb", [1, 16], F32, kind="ExternalInput").ap()
        self.ffn_w_in = nc.dram_tensor("ffn_w_in", [2, 2, D, 2 * DFF], F32, kind="ExternalInput").ap()
        self.ffn_w_out = nc.dram_tensor("ffn_w_out", [2, 2, DFF, D], F32, kind="ExternalInput").ap()
        self.a_w_in = nc.dram_tensor("a_w_in", [D, A_COLS], F32, kind="ExternalInput").ap()
        self.a_w_out = nc.dram_tensor("a_w_out", [D, D], F32, kind="ExternalInput").ap()
        self.b_w_in = nc.dram_tensor("b_w_in", [D, B_COLS], F32, kind="ExternalInput").ap()
        self.b_w_out = nc.dram_tensor("b_w_out", [D, D], F32, kind="ExternalInput").ap()
        self.out = nc.dram_tensor("out", [nseq, SEQ, D], F32, kind="ExternalOutput").ap()

        self.h = self.sb("h", [128, NCH, T], F32)
        self.NSLOT = 4
        self.wslot = [self.sb(f"wslot{i}", [128, 8, 512], BF16) for i in range(self.NSLOT)]
        self.wslot_next = 0
        self.ubuf = [self.sb(f"ubuf{i}", [128, NCH, 512], BF16) for i in range(2)]
        self.u_next = 0
        self.ubuf_small = self.sb("ubuf_small", [128, NCH, 16], BF16)
        AW = 7200
        self.arena = self.sb("arena", [128, AW], F32)
        ar = self.arena
        self.abuf = ar[:, 0:5632].bitcast(BF16).rearrange("p (f t) -> p f t", f=NF)
        self.sg = [ar[:, 5632 + i * 512: 5632 + (i + 1) * 512] for i in range(2)]
        self.abuf2 = ar[:, 6656:6832].bitcast(BF16).rearrange("p (f t) -> p f t", f=NF)
        self.qk = ar[:, 0:2048].bitcast(BF16).rearrange("p (c t) -> p c t", c=8)
        self.Vext = ar[:, 2048:4112].bitcast(BF16).rearrange("p (j h v) -> p j h v", j=4, h=8)
        self.zbuf = [ar[:, 4112 + i * 516: 4112 + i * 516 + 515] for i in range(2)]
        self.hsg = ar[:, 5144:7192].bitcast(BF16).rearrange("p (c t) -> p c t", c=8)
        self.arena_tokens = []
        self.PTa_s = None
        self.yt = self.sb("yt", [128, NCH, 512], F32)
        self.sq = self.sb("sq", [128, NCH, 512], BF16)
        self.yt2 = self.sb("yt2", [128, NCH, 16], F32)
        self.sq2 = self.sb("sq2", [128, NCH, 16], BF16)
        self.epsc = self.sb("epsc", [128, 1], F32)
        self.rstd = [self.sb(f"rstd{i}", [128, 512], F32) for i in range(2)]
        self.rstd_next = 0
        self.sgo = self.sb("sgo", [128, NCH, 512], BF16)
        self.arena2 = self.sb("arena2", [128, 3200], F32)
        a2 = self.arena2

        def carve(off, shape, dt):
            nel = int(np.prod(shape))
            words = nel if dt == F32 else (nel + 1) // 2
            v = a2[:, off:off + words]
            if dt != F32:
                v = v.bitcast(dt)
            if len(shape) == 2:
                names = "a b"
            elif len(shape) == 3:
                names = "a b c"
            else:
                names = "a b c d"
            if len(shape) > 1:
                kw = {nm: sz for nm, sz in zip(names.split(), shape)}
                v = v.rearrange("p (" + names + ") -> p " + names, **kw)
            return v
        self.lrep = carve(0, [8, 64], F32)
        self.bexp = [carve(512 + i * 128, [128], F32) for i in range(2)]
        self.qt = [carve(768 + i * 64, [128], BF16) for i in range(2)]
        self.Ka = [carve(896 + i * 64, [128], BF16) for i in range(2)]
        self.dn = [carve(1152 + i * 128, [128], F32) for i in range(2)]
        self.t1 = [carve(1408 + i * 128, [128], F32) for i in range(2)]
        self.Cst = carve(1664, [4, 129], F32)
        self.Cd = carve(2180, [129], F32)
        self.Cbf = carve(2312, [4, 128], BF16)
        self.nrep = carve(2568, [4, 128], BF16)
        self.halo = carve(2824, [8, 3], F32)
        self.gsb = carve(2848, [4, 16], F32)
        self.lneg = carve(2912, [4, 8], F32)
        self.avec = carve(2944, [4, 8], F32)
        self.gtmp = carve(2976, [4, 8], F32)
        self.qt_all = carve(0, [4, 128], BF16)
        self.kt_all = carve(256, [4, 128], BF16)
        self.Ka_all = carve(512, [4, 128], BF16)
        self.mPT = carve(768, [8, 128], BF16)
        self.nla = self.avec
        self.Sst = carve(0, [8, 128], F32)
        self.Sbf = carve(1024, [8, 128], BF16)
        self.qtl = [carve(1536 + i * 256, [512], BF16) for i in range(2)]
        self.ktl = [carve(2048 + i * 256, [512], BF16) for i in range(2)]
        self.kTs = [carve(2560 + i * 64, [128], BF16) for i in range(2)]
        self.decs = [carve(2688 + i * 8, [8], F32) for i in range(2)]
        self.Sd = carve(2704, [128], F32)
        self.PTa = carve(1536, [8, 128], BF16)
        self.kTa = carve(2048, [8, 128], BF16)
        self.decs_all = carve(2560, [8, 8], F32)
        self.arena2_tokens = []
        self.Vb = ar[:, 0:2048].bitcast(BF16).rearrange("p (j v) -> p j v", j=4)
        self.hsgb = ar[:, 2048:4096].bitcast(BF16).rearrange("p (c t) -> p c t", c=8)
        self.X = [ar[:, 2048 + i * 512: 2048 + (i + 1) * 512] for i in range(10)]
        self.PT = [self.sb(f"PT{i}", [128, 128], BF16) for i in range(2)]
        self.maskBD = self.sb("maskBD", [128, 128], F32)
        self.lbv = self.sb("lbv", [128, 8], F32)
        self.oml = self.sb("oml", [128, 8], F32)
        self.ones_f = self.sb("ones_f", [128, 64], F32)
        self.maskT = self.sb("maskT", [128, 128], F32)
        self.identb = self.sb("identb", [128, 128], BF16)
        self.gateb_bc = self.sb("gateb_bc", [128, 16], F32)
        self.sg_next = 0
        self.tmp = [self.sb(f"tmp{i}", [128, 512], F32) for i in range(2)]
        self.tmp_next = 0
        self.ptmp_next = 0
        self.xin = [self.sb(f"xin{i}", [128, D], F32) for i in range(2)]
        self.xin_next = 0
        self.ident = self.sb("ident", [128, 128], F32)
        self.ones_bf = self.sb("ones_bf", [128, 128], BF16)
        self.pv_raw = self.sb("pv_raw", [128, 2, 128], F32)
        self.pv = self.sb("pv", [128, 256], F32)
        self.gnh = self.sb("gnh", [128, 2, 6, NCH], F32)
        self.psum = [self.ps(f"psb{i}", [128, 512]) for i in range(8)]
        self.psum2_bf = self.psum[2][:, 0:512].bitcast(BF16)
        self.psum6_bf = self.psum[6][:, 0:512].bitcast(BF16)

        self.prologue()
        for s in range(nseq):
            self.load_seq(s)
            for st in self.stages:
                if st.startswith("ffn"):
                    layer = int(st[-1])
                    which = 0 if st[4] == "a" else 1
                    self.ffn_stage(layer, which)
                elif st == "mlstm":
                    self.mlstm_stage()
                elif st == "hgrn2":
                    self.hgrn2_stage()
            self.store_seq(s)
        self.T.finalize_and_emit()
        return nc

    def gain(self, layer, j, c):
        r = (layer * 6 + j) * 8 + c
        return self.pv[:, r:r + 1]

    def uid(self):
        self._uid += 1
        return self._uid

    def prologue(self):
        T_ = self.T
        nc = self.nc
        ident, ones_bf = self.ident, self.ones_bf
        T_.pool(lambda e: e.memset(ident[:], 0.0), writes=["ident"])
        T_.pool(lambda e: e.affine_select(out=ident[:], in_=ident[:], compare_op=ALU.not_equal, fill=1.0,
                                          base=0, pattern=[[-1, 128]], channel_multiplier=1),
                reads=["ident"], writes=["ident"])
        T_.pool(lambda e: e.memset(ones_bf[:], 1.0), writes=["ones"])
        T_.pool(lambda e: e.memset(self.arena[:], 0.0), writes=["arena_init"])
        self.arena_tokens = ["arena_init"]
        T_.pool(lambda e: e.memset(self.epsc[:], EPS), writes=["epsc"])
        maskT, identb = self.maskT, self.identb
        T_.pool(lambda e: e.memset(maskT[:], 1.0), writes=["maskT"])
        T_.pool(lambda e: e.affine_select(out=maskT[:], in_=maskT[:], compare_op=ALU.is_ge, fill=0.0,
                                          base=0, pattern=[[1, 128]], channel_multiplier=-1),
                reads=["maskT"], writes=["maskT"])
        T_.pool(lambda e: e.tensor_copy(identb[:], ident[:]), reads=["ident"], writes=["identb"])
        T_.pool(lambda e: e.tensor_copy(self.maskBD[:], maskT[:]), reads=["maskT"], writes=["maskBD"])
        T_.pool(lambda e: e.memset(self.maskBD[0:64, 64:128], 0.0), reads=["maskBD"], writes=["maskBD"])
        T_.pool(lambda e: e.memset(self.ones_f[:], 1.0), writes=["ones_f"])
        T_.dma("sp", lambda e: e.dma_start(out=self.gateb_bc[:], in_=self.gateb.partition_broadcast(128)),
               key="gateb", writes=["gateb"])
        pv_raw, pv = self.pv_raw, self.pv
        T_.dma("sp", lambda e: e.dma_start(out=pv_raw[:], in_=self.pvec.rearrange("(a p) f -> p a f", p=128)),
               key="pv_raw", writes=["pv_raw"])
        for a in range(2):
            pst = self.psum[a]
            T_.pe(lambda e, a=a, pst=pst: e.transpose(pst[:, 0:128], pv_raw[:, a, :], ident[:]),
                  reads=["pv_raw", "ident"], writes=[("ps", a)])
            T_.dve(lambda e, a=a, pst=pst: e.tensor_copy(pv[:, a * 128:(a + 1) * 128], pst[:, 0:128]),
                   reads=[("ps", a)], writes=["pv"])
        gnh = self.gnh
        T_.dve(lambda e: e.tensor_scalar(gnh[:].rearrange("p l j c -> p (l j c)"), pv[:, 0:96], 0.5, None, ALU.mult),
               reads=["pv"], writes=["gnh"])

    def load_seq(self, s):
        T_ = self.T
        h = self.h
        ident = self.ident
        nblk = SEQ // 128
        for tb in range(-1, nblk):
            xi = self.xin_next
            self.xin_next ^= 1
            xin = self.xin[xi]
            if tb < 0:
                np_, col0 = NMETA, 0
                src = self.meta[:, :]
            else:
                np_, col0 = 128, NMETA + tb * 128
                src = self.x[s, tb * 128:(tb + 1) * 128, :]
            T_.dma("sp", lambda e, xin=xin, src=src, np_=np_: e.dma_start(out=xin[0:np_, :], in_=src),
                   key=("xin", xi), writes=[("xin", xi)])
            for half in range(2):
                pb = 6 + half
                pst = self.psum[pb]
                for cc in range(4):
                    c = half * 4 + cc
                    T_.pe(lambda e, pst=pst, cc=cc, c=c, xin=xin, np_=np_:
                          e.transpose(pst[:, cc * 128:cc * 128 + np_], xin[0:np_, c * 128:(c + 1) * 128],
                                      ident[0:np_, 0:np_]),
                          reads=[("xin", xi), "ident"], writes=[("ps", pb)])
                eng = T_.dve if half == 0 else T_.act
                if half == 0:
                    T_.dve(lambda e, pst=pst, half=half, col0=col0, np_=np_:
                           e.tensor_copy(h[:, half * 4:half * 4 + 4, col0:col0 + np_],
                                         pst[:].rearrange("p (c t) -> p c t", c=4)[:, :, 0:np_]),
                           reads=[("ps", pb)], writes=[("h", c_, "io") for c_ in range(half * 4, half * 4 + 4)])
                else:
                    T_.act(lambda e, pst=pst, half=half, col0=col0, np_=np_:
                           e.activation(out=h[:, half * 4:half * 4 + 4, col0:col0 + np_],
                                        in_=pst[:].rearrange("p (c t) -> p c t", c=4)[:, :, 0:np_], func=AF.Copy),
                           reads=[("ps", pb)], writes=[("h", c_, "io") for c_ in range(half * 4, half * 4 + 4)])
        self.h_io_to_tiles()

    TILES = [(0, 512), (512, 512), (1024, 512), (1536, 512), (2048, 16)]

    def htok(self, c, ti):
        return ("h", c, ti)

    def h_io_to_tiles(self):
        self.T.alias([self.htok(c, ti) for c in range(NCH) for ti in range(len(self.TILES))],
                     [("h", c, "io") for c in range(NCH)])

    def h_tiles_to_io(self):
        self.T.alias([("h", c, "io") for c in range(NCH)],
                     [self.htok(c, ti) for c in range(NCH) for ti in range(len(self.TILES))])

    def store_seq(self, s):
        T_ = self.T
        h = self.h
        ident = self.ident
        self.h_tiles_to_io()
        nblk = SEQ // 128
        for tb in range(nblk):
            xi = self.xin_next
            self.xin_next ^= 1
            xout = self.xin[xi]
            col0 = NMETA + tb * 128
            for half in range(2):
                pb = 6 + half
                pst = self.psum[pb]
                for cc in range(4):
                    c = half * 4 + cc
                    T_.pe(lambda e, pst=pst, cc=cc, c=c, col0=col0:
                          e.transpose(pst[:, cc * 128:(cc + 1) * 128], h[:, c, col0:col0 + 128], ident[:]),
                          reads=["ident"], writes=[("ps", pb), ("h", c, "io")])
                if half == 0:
                    T_.dve(lambda e, pst=pst, xout=xout, half=half:
                           e.tensor_copy(xout[:, half * 512:(half + 1) * 512], pst[:]),
                           reads=[("ps", pb)], writes=[("xin", xi)])
                else:
                    T_.act(lambda e, pst=pst, xout=xout, half=half:
                           e.activation(out=xout[:, half * 512:(half + 1) * 512], in_=pst[:], func=AF.Copy),
                           reads=[("ps", pb)], writes=[("xin", xi)])
            T_.dma("sp", lambda e, xout=xout, s=s, tb=tb:
                   e.dma_start(out=self.out[s, tb * 128:(tb + 1) * 128, :], in_=xout[:]),
                   key=("xin", xi), reads=[("xin", xi)], writes=[("out", s, tb)])
        T_.add("sp", lambda e: e.nop(), reads=[("out", s, tb) for tb in range(nblk)])

    def wload(self, src_ap, nk, ncols, slot=None):
        if slot is None:
            si = self.wslot_next
            self.wslot_next = (si + 1) % self.NSLOT
        else:
            si = slot
        slot = self.wslot[si]
        tok = ("w", si)
        src = src_ap.rearrange("(k p) f -> p k f", p=128)
        self.T.dma("pool", lambda e, slot=slot, src=src, nk=nk, ncols=ncols:
                   e.dma_start(out=slot[:, 0:nk, 0:ncols], in_=src), key=tok, writes=[tok])
        return slot, tok

    def wplan_reset(self, plan):
        self._wplan = plan
        self._wgot = []

    def wget(self, idx):
        while len(self._wgot) < min(len(self._wplan), idx + 3):
            src, nk, ncols = self._wplan[len(self._wgot)]
            self._wgot.append(self.wload(src, nk, ncols))
        return self._wgot[idx]

    def rstd_from_sq(self, sq_tokens, n, psb, dim_chunks=NCH):
        T_ = self.T
        pst = self.psum[psb]
        sq, ones = self.sq, self.ones_bf
        for c in range(dim_chunks):
            T_.pe(lambda e, c=c: e.matmul(pst[:, 0:n], lhsT=ones[:], rhs=sq[:, c, 0:n],
                                          start=(c == 0), stop=(c == dim_chunks - 1)),
                  reads=["ones", sq_tokens[c]], writes=[("ps", psb)])
        ri = self.rstd_next
        self.rstd_next ^= 1
        rs = self.rstd[ri]
        rtok = ("rstd", ri)
        T_.act(lambda e: e.activation(out=rs[:, 0:n], in_=pst[:, 0:n], func=AF.Ln,
                                      scale=1.0 / (128 * dim_chunks), bias=self.eps_ap()),
               reads=[("ps", psb), "epsc"], writes=[rtok])
        T_.act(lambda e: e.activation(out=rs[:, 0:n], in_=rs[:, 0:n], func=AF.Exp, scale=-0.5), reads=[rtok], writes=[rtok])
        return rs, rtok

    def eps_ap(self):
        return self.epsc[:, 0:1]

    def prenorm(self, layer, j, ti, psb):
        T_ = self.T
        h, sq = self.h, self.sq
        t0, n = self.TILES[ti]
        for c in range(NCH):
            T_.act(lambda e, c=c: e.activation(out=sq[:, c, 0:n], in_=h[:, c, t0:t0 + n], func=AF.Square),
                   reads=[self.htok(c, ti)], writes=[("sq", c)])
        rs, rtok = self.rstd_from_sq([("sq", c) for c in range(NCH)], n, psb)
        if n <= 16:
            ui, ub = 2, self.ubuf_small
        else:
            ui = self.u_next
            self.u_next ^= 1
            ub = self.ubuf[ui]
        for c in range(NCH):
            T_.dve(lambda e, c=c: e.scalar_tensor_tensor(out=ub[:, c, 0:n], in0=h[:, c, t0:t0 + n],
                                                         scalar=self.gain(layer, j, c), in1=rs[:, 0:n],
                                                         op0=ALU.mult, op1=ALU.mult),
                   reads=[self.htok(c, ti), rtok, "pv"], writes=[("u", ui, c)])
        return ub, ui

    def postnorm_residual(self, ti, psb):
        T_ = self.T
        h, yt = self.h, self.yt
        t0, n = self.TILES[ti]
        rs, rtok = self.rstd_from_sq([("sq", c) for c in range(NCH)], n, psb)
        for c in range(NCH):
            tix = self.tmp_next
            self.tmp_next ^= 1
            tmp = self.tmp[tix]
            T_.dve(lambda e, c=c, tmp=tmp: e.tensor_tensor(out=tmp[:, 0:n], in0=yt[:, c, 0:n], in1=rs[:, 0:n], op=ALU.mult),
                   reads=[("yt", c), rtok], writes=[("tmp", tix)])
            T_.dve(lambda e, c=c, tmp=tmp: e.tensor_tensor(out=h[:, c, t0:t0 + n], in0=h[:, c, t0:t0 + n], in1=tmp[:, 0:n], op=ALU.add),
                   reads=[("tmp", tix), self.htok(c, ti)], writes=[self.htok(c, ti)])

    def arena_handoff(self, new_tokens):
        self.T.alias(new_tokens, self.arena_tokens)
        self.arena_tokens = list(new_tokens)

    def arena2_handoff(self, new_tokens):
        self.T.alias(new_tokens, self.arena2_tokens)
        self.arena2_tokens = list(new_tokens)

    def ffn_stage(self, layer, which):
        T_ = self.T
        self.arena_handoff([("a", fl, f) for fl in (True, False) for f in range(NF)] + [("sg", 0), ("sg", 1)])
        j_pre, j_post = (0, 1) if which == 0 else (4, 5)
        w_in = self.ffn_w_in[layer, which]
        w_out = self.ffn_w_out[layer, which]
        abuf, yt, sq = self.abuf, self.yt, self.sq
        ntile = len(self.TILES)
        groups = [[i] for i in range(ntile)]
        if ntile > 1 and self.TILES[-1][1] <= 16:
            groups = groups[:-2] + [[ntile - 2, ntile - 1]]
        plan = []
        for _g in groups:
            for js in range(6):
                nf_ = 4 if js < 5 else 2
                plan.append((w_in[:, js * 512: js * 512 + nf_ * 128], 8, nf_ * 128))
                plan.append((w_in[:, DFF + js * 512: DFF + js * 512 + nf_ * 128], 8, nf_ * 128))
            for dh_ in range(2):
                for ks_ in range(3):
                    nk_ = 8 if ks_ < 2 else 6
                    plan.append((w_out[ks_ * 1024: ks_ * 1024 + nk_ * 128, dh_ * 512:(dh_ + 1) * 512], nk_, 512))
        self.wplan_reset(plan)
        ubs = {}
        for ti in groups[0]:
            ubs[ti] = self.prenorm(layer, j_pre, ti, psb=5)
        pending_post = []
        for gi, grp in enumerate(groups):
            wbase = 18 * gi
            for jslab in range(6):
                if jslab == 1:
                    for g_ in pending_post:
                        next(g_, None)
                if jslab == 3 and gi + 1 < len(groups):
                    for ti in groups[gi + 1]:
                        ubs[ti] = self.prenorm(layer, j_pre, ti, psb=5)
                nf = 4 if jslab < 5 else 2
                gs, gtok = self.wget(wbase + 2 * jslab)
                us, utok = self.wget(wbase + 2 * jslab + 1)
                for fi in range(nf):
                    f = jslab * 4 + fi
                    if jslab >= 1 and pending_post:
                        if next(pending_post[0], "done") == "done":
                            pending_post.pop(0)
                            if pending_post:
                                next(pending_post[0], None)
                    for ti in grp:
                        t0, n = self.TILES[ti]
                        ub, ui = ubs[ti]
                        par = self.uid() % 2
                        gb, ubk = 0 + par * 2, 1 + par * 2
                        gps, ups = self.psum[gb], self.psum[ubk]
                        for k in range(8):
                            T_.pe(lambda e, k=k, fi=fi, gs=gs, ub=ub, n=n, gps=gps:
                                  e.matmul(gps[:, 0:n], lhsT=gs[:, k, fi * 128:(fi + 1) * 128], rhs=ub[:, k, 0:n],
                                           start=(k == 0), stop=(k == 7)),
                                  reads=[gtok, ("u", ui, k)], writes=[("ps", gb)])
                        for k in range(8):
                            T_.pe(lambda e, k=k, fi=fi, us=us, ub=ub, n=n, ups=ups:
                                  e.matmul(ups[:, 0:n], lhsT=us[:, k, fi * 128:(fi + 1) * 128], rhs=ub[:, k, 0:n],
                                           start=(k == 0), stop=(k == 7)),
                                  reads=[utok, ("u", ui, k)], writes=[("ps", ubk)])
                        sgi = self.sg_next
                        self.sg_next ^= 1
                        sg = self.sg[sgi]
                        T_.act(lambda e, sg=sg, gps=gps, n=n: e.activation(out=sg[:, 0:n], in_=gps[:, 0:n], func=AF.Silu),
                               reads=[("ps", gb)], writes=[("sg", sgi)])
                        acol = 0 if ti == grp[0] else 512 - 16
                        ab = abuf if ti == grp[0] else self.abuf2
                        T_.dve(lambda e, sg=sg, ups=ups, n=n, f=f, ab=ab:
                               e.tensor_tensor(out=ab[:, f, 0:n], in0=sg[:, 0:n], in1=ups[:, 0:n], op=ALU.mult),
                               reads=[("sg", sgi), ("ps", ubk)], writes=[("a", ti == grp[0], f)])
            for dh in range(2):
                ybank = {}
                for ks in range(3):
                    nk = 8 if ks < 2 else 6
                    ws, wtok = self.wget(wbase + 12 + dh * 3 + ks)
                    for ti in grp:
                        t0, n = self.TILES[ti]
                        ab = abuf if ti == grp[0] else self.abuf2
                        for di in range(4):
                            yb = 4 + di if ti == grp[0] else di
                            yps = self.psum[yb]
                            for kk in range(nk):
                                fch = ks * 8 + kk
                                T_.pe(lambda e, kk=kk, di=di, ws=ws, ab=ab, fch=fch, n=n, yps=yps, ks=ks, nk=nk:
                                      e.matmul(yps[:, 0:n], lhsT=ws[:, kk, di * 128:(di + 1) * 128], rhs=ab[:, fch, 0:n],
                                               start=(ks == 0 and kk == 0), stop=(ks == 2 and kk == nk - 1)),
                                      reads=[wtok, ("a", ti == grp[0], fch)], writes=[("ps", yb)])
                for ti in grp:
                    t0, n = self.TILES[ti]
                    first = ti == grp[0]
                    ytt = yt if first else self.yt2
                    sqq = sq if first else self.sq2
                    for di in range(4):
                        d = dh * 4 + di
                        yb = 4 + di if first else di
                        yps = self.psum[yb]
                        T_.act(lambda e, d=d, yps=yps, n=n, sqq=sqq: e.activation(out=sqq[:, d, 0:n], in_=yps[:, 0:n], func=AF.Square),
                               reads=[("ps", yb)], writes=[("sq" if first else "sq2", d)])
                        T_.act(lambda e, d=d, yps=yps, n=n, ytt=ytt: e.activation(out=ytt[:, d, 0:n], in_=yps[:, 0:n], func=AF.Copy,
                                                                         scale=self.gnh[:, layer, j_post, d:d + 1]),
                               reads=[("ps", yb), "gnh"], writes=[("yt" if first else "yt2", d)])
            for ti in grp:
                first = ti == grp[0]
                pending_post.append(self.postnorm_gen(ti, 5, first))
        for g_ in pending_post:
            for _ in g_:
                pass

    def postnorm_residual2(self, ti, psb, first):
        for _ in self.postnorm_gen(ti, psb, first):
            pass

    def postnorm_gen(self, ti, psb, first):
        T_ = self.T
        h = self.h
        yt = self.yt if first else self.yt2
        sq = self.sq if first else self.sq2
        sqn = "sq" if first else "sq2"
        ytn = "yt" if first else "yt2"
        t0, n = self.TILES[ti]
        pst = self.psum[psb]
        ones = self.ones_bf
        for c in range(NCH):
            T_.pe(lambda e, c=c: e.matmul(pst[:, 0:n], lhsT=ones[:], rhs=sq[:, c, 0:n],
                                          start=(c == 0), stop=(c == NCH - 1)),
                  reads=["ones", (sqn, c)], writes=[("ps", psb)])
        ri = self.rstd_next
        self.rstd_next ^= 1
        rs = self.rstd[ri]
        rtok = ("rstd", ri)
        T_.act(lambda e: e.activation(out=rs[:, 0:n], in_=pst[:, 0:n], func=AF.Ln,
                                      scale=1.0 / D, bias=self.eps_ap()),
               reads=[("ps", psb), "epsc"], writes=[rtok])
        T_.act(lambda e: e.activation(out=rs[:, 0:n], in_=rs[:, 0:n], func=AF.Exp, scale=-0.5), reads=[rtok], writes=[rtok])
        yield
        for c in range(NCH):
            tix = self.tmp_next
            self.tmp_next ^= 1
            tmp = self.tmp[tix]
            T_.dve(lambda e, c=c, tmp=tmp: e.tensor_tensor(out=tmp[:, 0:n], in0=yt[:, c, 0:n], in1=rs[:, 0:n], op=ALU.mult),
                   reads=[(ytn, c), rtok], writes=[("tmp", tix)])
            T_.dve(lambda e, c=c, tmp=tmp: e.tensor_tensor(out=h[:, c, t0:t0 + n], in0=h[:, c, t0:t0 + n], in1=tmp[:, 0:n], op=ALU.add),
                   reads=[("tmp", tix), self.htok(c, ti)], writes=[self.htok(c, ti)])
            yield

    def mlstm_stage(self):
        T_ = self.T
        w_in, w_out = self.a_w_in, self.a_w_out
        h, sq, yt = self.h, self.sq, self.yt
        qk, Vext, hsg, sgo = self.qk, self.Vext, self.hsg, self.sgo
        ident, identb, maskT, ones = self.ident, self.identb, self.maskT, self.ones_bf
        Cst, Cbf, nrep, Cd = self.Cst, self.Cbf, self.nrep, self.Cd
        gsb, lneg, avec, gtmp, lrep = self.gsb, self.lneg, self.avec, self.gtmp, self.lrep
        pv = self.pv
        self.arena_handoff([("qk", c) for c in range(8)] + [("V", j, hf) for j in range(4) for hf in range(2)]
                           + [("zb", i) for i in range(2)] + [("hsg", c) for c in range(8)])
        self.arena2_handoff(["Call", "Cbfall", "nrepall", ("qt_all", 0), ("kt_all", 0), ("Ka_all", 0), ("mPT", 0, 0), ("mPT", 0, 1)]
                            + [("halo", c) for c in range(8)]
                            + [(nm, j) for nm in ("gsb", "lneg", "nla", "gtmp") for j in range(4)])
        T_.alias([("EA", 0, 0), ("EA", 0, 1)], [("xin", 0)])
        T_.alias([("rep", 0, 0), ("rep", 0, 1)], [("xin", 1)])
        T_.dve(lambda e: e.memset(Cst[:], 0.0), writes=["Call"])
        T_.dve(lambda e: e.memset(Cbf[:], 0.0), writes=["Cbfall"])
        T_.dve(lambda e: e.memset(nrep[:], 0.0), writes=["nrepall"])
        T_.dve(lambda e: e.memset(self.halo[:], 0.0), writes=[("halo", c) for c in range(8)])
        T_.dve(lambda e: e.memset(Vext[:], 1.0), writes=[("V", j, hf) for j in range(4) for hf in range(2)])
        rot = {"zb": 0, "bexp": 0, "qt": 0, "Ka": 0, "PT": 0, "dn": 0, "t1": 0, "pp": 0, "pv": 0}

        def nxt(k, m=2):
            v = rot[k]
            rot[k] = (v + 1) % m
            return v

        for ti, (t0, n) in enumerate(self.TILES):
            self._mlstm_tile(ti, t0, n, nxt)
        T_.alias([("xin", 0)], [("EA", 0, 0), ("EA", 0, 1)])
        T_.alias([("xin", 1)], [("rep", 0, 0), ("rep", 0, 1)])

    def _mlstm_tile(self, ti, t0, n, nxt):
        T_ = self.T
        w_in, w_out = self.a_w_in, self.a_w_out
        h, sq, yt = self.h, self.sq, self.yt
        qk, Vext, hsg, sgo = self.qk, self.Vext, self.hsg, self.sgo
        ident, identb, maskT, ones = self.ident, self.identb, self.maskT, self.ones_bf
        Cst, Cbf, nrep, Cd = self.Cst, self.Cbf, self.nrep, self.Cd
        gsb, lneg, avec, gtmp, lrep = self.gsb, self.lneg, self.avec, self.gtmp, self.lrep
        pv = self.pv
        if True:
            ub, ui = self.prenorm(0, 2, ti, psb=5)
            utoks = [("u", ui, k) for k in range(8)]
            nchunk = (n + 127) // 128
            for part in range(2):
                slab, stok = self.wload(w_in[:, part * 512:(part + 1) * 512], 8, 512)
                for cc in range(4):
                    c8 = part * 4 + cc
                    pb = nxt("pp")
                    pst = self.psum[pb]
                    for k in range(8):
                        T_.pe(lambda e, k=k, cc=cc, slab=slab, pst=pst:
                              e.matmul(pst[:, 0:n], lhsT=slab[:, k, cc * 128:(cc + 1) * 128], rhs=ub[:, k, 0:n],
                                       start=(k == 0), stop=(k == 7)),
                              reads=[stok, utoks[k]], writes=[("ps", pb)])
                    zi = nxt("zb")
                    zb = self.zbuf[zi]
                    T_.act(lambda e, zb=zb, pst=pst: e.activation(out=zb[:, 3:3 + n], in_=pst[:, 0:n], func=AF.Copy),
                           reads=[("ps", pb)], writes=[("zb", zi)])
                    T_.act(lambda e, zb=zb, c8=c8: e.activation(out=zb[:, 0:3], in_=self.halo[:, c8, :], func=AF.Copy),
                           reads=[("halo", c8)], writes=[("zb", zi)])
                    T_.act(lambda e, zb=zb, c8=c8: e.activation(out=self.halo[:, c8, :], in_=zb[:, n:n + 3], func=AF.Copy),
                           reads=[("zb", zi)], writes=[("halo", c8)])
                    tix = self.tmp_next
                    self.tmp_next ^= 1
                    acc = self.tmp[tix]
                    wcol = lambda kk, c8=c8: pv[:, 96 + kk * 8 + c8: 96 + kk * 8 + c8 + 1]
                    bcol = pv[:, 128 + c8:128 + c8 + 1]
                    T_.dve(lambda e, zb=zb, acc=acc, wcol=wcol, bcol=bcol:
                           e.tensor_scalar(acc[:, 0:n], zb[:, 3:3 + n], wcol(3), bcol, ALU.mult, ALU.add),
                           reads=[("zb", zi), "pv"], writes=[("tmp", tix)])
                    for kk in (2, 1, 0):
                        T_.dve(lambda e, zb=zb, acc=acc, wcol=wcol, kk=kk:
                               e.scalar_tensor_tensor(out=acc[:, 0:n], in0=zb[:, kk:kk + n], scalar=wcol(kk),
                                                      in1=acc[:, 0:n], op0=ALU.mult, op1=ALU.add),
                               reads=[("zb", zi), ("tmp", tix), "pv"], writes=[("tmp", tix)])
                    T_.act(lambda e, acc=acc, c8=c8: e.activation(out=qk[:, c8, 0:n], in_=acc[:, 0:n], func=AF.Silu),
                           reads=[("tmp", tix)], writes=[("qk", c8)])
            for half in range(2):
                slab, stok = self.wload(w_in[:, 2048 + half * 512: 2048 + (half + 1) * 512], 8, 512)
                for cc in range(4):
                    c8 = half * 4 + cc
                    pb = nxt("pp")
                    pst = self.psum[pb]
                    for k in range(8):
                        T_.pe(lambda e, k=k, cc=cc, slab=slab, pst=pst:
                              e.matmul(pst[:, 0:n], lhsT=slab[:, k, cc * 128:(cc + 1) * 128], rhs=ub[:, k, 0:n],
                                       start=(k == 0), stop=(k == 7)),
                              reads=[stok, utoks[k]], writes=[("ps", pb)])
                    T_.act(lambda e, pst=pst, c8=c8: e.activation(out=sgo[:, c8, 0:n], in_=pst[:, 0:n], func=AF.Sigmoid),
                           reads=[("ps", pb)], writes=[("sgo", c8)])
            for half in range(2):
                slab, stok = self.wload(w_in[:, 1024 + half * 512: 1024 + (half + 1) * 512], 8, 512)
                for j in range(nchunk):
                    cn = min(128, n - j * 128)
                    pb = 2 + nxt("pv")
                    pst = self.psum[pb]
                    for k in range(8):
                        T_.pe(lambda e, k=k, j=j, cn=cn, slab=slab, pst=pst:
                              e.matmul(pst[0:cn, 0:512], lhsT=ub[:, k, j * 128:j * 128 + cn], rhs=slab[:, k, 0:512],
                                       start=(k == 0), stop=(k == 7)),
                              reads=[stok, utoks[k]], writes=[("ps", pb)])
                    T_.act(lambda e, j=j, cn=cn, half=half, pst=pst:
                           e.activation(out=Vext[0:cn, j, half * 4:(half + 1) * 4, 0:128],
                                        in_=pst[0:cn, :].rearrange("p (h v) -> p h v", h=4), func=AF.Copy),
                           reads=[("ps", pb)], writes=[("V", j, half)])
            gslab, gstok = self.wload(w_in[:, 3072:3088], 8, 16)
            for j in range(nchunk):
                cn = min(128, n - j * 128)
                pst = self.psum[3]
                for k in range(8):
                    T_.pe(lambda e, k=k, j=j, cn=cn, pst=pst:
                          e.matmul(pst[0:cn, j * 16:(j + 1) * 16], lhsT=ub[:, k, j * 128:j * 128 + cn], rhs=gslab[:, k, 0:16],
                                   start=(k == 0), stop=(k == 7)),
                          reads=[gstok, utoks[k]], writes=[("ps", 3)])
                T_.dve(lambda e, j=j, cn=cn, pst=pst:
                       e.tensor_tensor(out=gsb[0:cn, j, :], in0=pst[0:cn, j * 16:(j + 1) * 16], in1=self.gateb_bc[0:cn, :], op=ALU.add),
                       reads=[("ps", 3), "gateb"], writes=[("gsb", j)])
                T_.act(lambda e, j=j, cn=cn: e.activation(out=gtmp[0:cn, j, :], in_=gsb[0:cn, j, 8:16], func=AF.Exp, scale=-1.0),
                       reads=[("gsb", j)], writes=[("gtmp", j)])
                T_.act(lambda e, j=j, cn=cn: e.activation(out=lneg[0:cn, j, :], in_=gtmp[0:cn, j, :], func=AF.Ln, bias=1.0),
                       reads=[("gtmp", j)], writes=[("lneg", j)])
                T_.pe(lambda e, j=j, cn=cn, pst=pst: e.matmul(pst[0:cn, 64 + 8 * j:72 + 8 * j], lhsT=self.maskT[0:cn, 0:cn], rhs=lneg[0:cn, j, :],
                                                      start=True, stop=True),
                      reads=["maskT", ("lneg", j)], writes=[("ps", 3)])
                T_.dve(lambda e, j=j, cn=cn, pst=pst: e.scalar_tensor_tensor(out=self.nla[0:cn, j, :], in0=pst[0:cn, 64 + 8 * j:72 + 8 * j], scalar=-1.0,
                                                                     in1=gsb[0:cn, j, 0:8], op0=ALU.mult, op1=ALU.subtract),
                       reads=[("ps", 3), ("gsb", j)], writes=[("nla", j)])
            T_.alias(self._mlstm_set_tokens(1), [("yt", d) for d in range(NCH)])
            self._mlstm_front(0, min(128, n), 0, 0)
            for j in range(nchunk):
                if j + 1 < nchunk:
                    self._mlstm_front(j + 1, min(128, n - (j + 1) * 128), (j + 1) * 128, (j + 1) % 2)
                self._mlstm_back(j, min(128, n - j * 128), j * 128, j % 2)
            T_.alias([("yt", d) for d in range(NCH)], self._mlstm_set_tokens(1))
            self._mlstm_outproj(ti, n)

    def _mlstm_chunk(self, j, cn, c0, nxt):
        T_ = self.T
        qk, Vext, hsg, sgo = self.qk, self.Vext, self.hsg, self.sgo
        ident, identb, maskT, ones = self.ident, self.identb, self.maskT, self.ones_bf
        Cst, Cbf, nrep, Cd = self.Cst, self.Cbf, self.nrep, self.Cd
        gsb, lneg, avec, gtmp, lrep = self.gsb, self.lneg, self.avec, self.gtmp, self.lrep
        if True:
            if True:
                pst4 = self.psum[4]
                T_.pe(lambda e, j=j, cn=cn: e.matmul(pst4[0:cn, 64:72], lhsT=maskT[0:cn, 0:cn], rhs=lneg[0:cn, j, :], start=True, stop=True),
                      reads=["maskT", ("lneg", j)], writes=[("ps", 4)])
                T_.dve(lambda e, j=j, cn=cn: e.tensor_tensor(out=gtmp[0:cn, j, :], in0=pst4[0:cn, 64:72], in1=gsb[0:cn, j, 0:8], op=ALU.add),
                       reads=[("ps", 4), ("gsb", j)], writes=[("gtmp", j)])
                T_.act(lambda e, j=j, cn=cn: e.activation(out=avec[0:cn, j, :], in_=gtmp[0:cn, j, :], func=AF.Exp),
                       reads=[("gtmp", j)], writes=[("avec", j)])
                T_.dve(lambda e, j=j, cn=cn: e.tensor_copy(lrep[0:cn, :, :], lneg[0:cn, j, :].unsqueeze(2).to_broadcast([cn, 8, 64])),
                       reads=[("lneg", j)], writes=["lrep"])
                for hp in range(4):
                    self._mlstm_pair(j, cn, c0, hp, nxt)

    def _mlstm_pair(self, j, cn, c0, hp, nxt):
        T_ = self.T
        qk, Vext, hsg, sgo = self.qk, self.Vext, self.hsg, self.sgo
        ident, identb, maskT, ones = self.ident, self.identb, self.maskT, self.ones_bf
        Cst, Cbf, nrep, Cd = self.Cst, self.Cbf, self.nrep, self.Cd
        gsb, lneg, avec, gtmp, lrep = self.gsb, self.lneg, self.avec, self.gtmp, self.lrep
        if True:
            if True:
                if True:
                    fbb = hp % 2
                    fbp = self.psum[fbb]
                    T_.pe(lambda e, hp=hp, cn=cn, fbp=fbp: e.matmul(fbp[:, 0:cn], lhsT=lrep[0:cn, 2 * hp:2 * hp + 2, :].rearrange("p a b -> p (a b)"),
                                                           rhs=maskT[0:cn, 0:cn], start=True, stop=True),
                          reads=["lrep", "maskT"], writes=[("ps", fbb)])
                    bi = nxt("bexp")
                    bexp = self.bexp[bi]
                    T_.act(lambda e, bexp=bexp, cn=cn, fbp=fbp: e.activation(out=bexp[:, 0:cn], in_=fbp[:, 0:cn], func=AF.Exp, scale=-1.0),
                           reads=[("ps", fbb)], writes=[("bexp", bi)])
                    qi = nxt("qt")
                    qt = self.qt[qi]
                    T_.dve(lambda e, qt=qt, bexp=bexp, hp=hp, cn=cn, c0=c0:
                           e.scalar_tensor_tensor(out=qt[:, 0:cn], in0=qk[:, hp, c0:c0 + cn], scalar=0.125, in1=bexp[:, 0:cn],
                                                  op0=ALU.mult, op1=ALU.mult),
                           reads=[("qk", hp), ("bexp", bi)], writes=[("qt", qi)])
                    kps = self.psum2_bf[:, 0:128]
                    T_.pe(lambda e, hp=hp, cn=cn, c0=c0, kps=kps:
                          e.transpose(kps[0:cn, :], qk[:, 4 + hp, c0:c0 + cn], identb[:]),
                          reads=[("qk", 4 + hp), "identb"], writes=[("ps", 2)])
                    kai = nxt("Ka")
                    Ka = self.Ka[kai]
                    T_.dve(lambda e, Ka=Ka, kps=kps, hp=hp, j=j, cn=cn:
                           e.tensor_tensor(out=Ka[0:cn, :].rearrange("p (a b) -> p a b", a=2),
                                           in0=kps[0:cn, :].rearrange("p (a b) -> p a b", a=2),
                                           in1=avec[0:cn, j, 2 * hp:2 * hp + 2].unsqueeze(2).to_broadcast([cn, 2, 64]), op=ALU.mult),
                           reads=[("ps", 2), ("avec", j)], writes=[("Ka", kai)])
                    ups = self.psum[3]
                    for ev in range(2):
                        self._mlstm_head(j, cn, c0, hp, ev, nxt, qt, qi, Ka, kai, ups)
                    self._mlstm_state(hp, cn, bexp, bi, ups)

    def _mlstm_head(self, j, cn, c0, hp, ev, nxt, qt, qi, Ka, kai, ups):
        T_ = self.T
        qk, Vext, hsg, sgo = self.qk, self.Vext, self.hsg, self.sgo
        ident, identb, maskT, ones = self.ident, self.identb, self.maskT, self.ones_bf
        Cst, Cbf, nrep, Cd = self.Cst, self.Cbf, self.nrep, self.Cd
        avec = self.avec
        if True:
            if True:
                if True:
                    if True:
                        hh = 2 * hp + ev
                        r0 = 64 * ev
                        stb = 5 - ev
                        stp = self.psum[stb]
                        T_.pe(lambda e, hp=hp, cn=cn, c0=c0, r0=r0, ev=ev, qt=qt, stp=stp:
                              e.matmul(stp[0:cn, 128:128 + cn], lhsT=qk[r0:r0 + 64, 4 + hp, c0:c0 + cn],
                                       rhs=qt[r0:r0 + 64, 0:cn], start=True, stop=True),
                              reads=[("qk", 4 + hp), ("qt", qi)], writes=[("ps", stb)])
                        pi = nxt("PT")
                        PT = self.PT[pi]
                        T_.dve(lambda e, PT=PT, stp=stp, ev=ev, cn=cn, j=j, hh=hh:
                               e.scalar_tensor_tensor(out=PT[0:cn, 0:cn], in0=stp[0:cn, 128:128 + cn],
                                                      scalar=avec[0:cn, j, hh:hh + 1], in1=maskT[0:cn, 0:cn],
                                                      op0=ALU.mult, op1=ALU.mult),
                               reads=[("ps", stb), ("avec", j), "maskT"], writes=[("PT", pi)])
                        hpb = 6 + ev
                        hps = self.psum[hpb]
                        ho = 0
                        T_.pe(lambda e, PT=PT, cn=cn, j=j, hh=hh, hps=hps, ho=ho:
                              e.matmul(hps[:, ho:ho + cn], lhsT=Vext[0:cn, j, hh, 0:128], rhs=PT[0:cn, 0:cn], start=True, stop=False),
                              reads=[("V", j, hh // 4), ("PT", pi)], writes=[("ps", hpb)])
                        T_.pe(lambda e, cn=cn, hp=hp, r0=r0, qt=qt, hps=hps, ho=ho:
                              e.matmul(hps[:, ho:ho + cn], lhsT=Cbf[r0:r0 + 64, hp, :], rhs=qt[r0:r0 + 64, 0:cn], start=False, stop=True),
                              reads=[("Cbf", hp), ("qt", qi)], writes=[("ps", hpb)])
                        T_.pe(lambda e, PT=PT, cn=cn, hps=hps, ho=ho:
                              e.matmul(hps[:, ho + 128:ho + 128 + cn], lhsT=ones[0:cn, :], rhs=PT[0:cn, 0:cn], start=True, stop=False),
                              reads=["ones", ("PT", pi)], writes=[("ps", hpb)])
                        T_.pe(lambda e, cn=cn, hp=hp, r0=r0, qt=qt, hps=hps, ho=ho:
                              e.matmul(hps[:, ho + 128:ho + 128 + cn], lhsT=nrep[r0:r0 + 64, hp, :], rhs=qt[r0:r0 + 64, 0:cn], start=False, stop=True),
                              reads=[("nrep", hp), ("qt", qi)], writes=[("ps", hpb)])
                        di = nxt("dn")
                        dn = self.dn[di]
                        T_.act(lambda e, dn=dn, hps=hps, ho=ho, cn=cn:
                               e.activation(out=dn[:, 0:cn], in_=hps[:, ho + 128:ho + 128 + cn], func=AF.Abs),
                               reads=[("ps", hpb)], writes=[("dn", di)])
                        T_.dve(lambda e, dn=dn, cn=cn: e.tensor_scalar(dn[:, 0:cn], dn[:, 0:cn], 1.0, None, ALU.max),
                               reads=[("dn", di)], writes=[("dn", di)])
                        T_.dve(lambda e, dn=dn, cn=cn: e.reciprocal(dn[:, 0:cn], dn[:, 0:cn]), reads=[("dn", di)], writes=[("dn", di)])
                        t1i = nxt("t1")
                        t1 = self.t1[t1i]
                        T_.dve(lambda e, t1=t1, dn=dn, hps=hps, ho=ho, cn=cn:
                               e.tensor_tensor(out=t1[:, 0:cn], in0=hps[:, ho:ho + cn], in1=dn[:, 0:cn], op=ALU.mult),
                               reads=[("ps", hpb), ("dn", di)], writes=[("t1", t1i)])
                        T_.dve(lambda e, t1=t1, hh=hh, c0=c0, cn=cn:
                               e.tensor_tensor(out=hsg[:, hh, c0:c0 + cn], in0=t1[:, 0:cn], in1=sgo[:, hh, c0:c0 + cn], op=ALU.mult),
                               reads=[("t1", t1i), ("sgo", hh)], writes=[("hsg", hh)])
                        T_.pe(lambda e, Ka=Ka, cn=cn, j=j, hh=hh, r0=r0, ups=ups:
                              e.matmul(ups[r0:r0 + 64, 0:129], lhsT=Ka[0:cn, r0:r0 + 64], rhs=Vext[0:cn, j, hh, 0:129], start=True, stop=True),
                              reads=[("Ka", kai), ("V", j, hh // 4)], writes=[("ps", 3)])

    def _mlstm_state(self, hp, cn, bexp, bi, ups):
        T_ = self.T
        Cst, Cbf, nrep, Cd = self.Cst, self.Cbf, self.nrep, self.Cd
        if True:
            if True:
                if True:
                    dec = bexp[:, cn - 1:cn]
                    T_.dve(lambda e, hp=hp, dec=dec: e.tensor_scalar(Cd[:, :], Cst[:, hp, :], dec, None, ALU.mult),
                           reads=[("C", hp), ("bexp", bi)], writes=["Cd"])
                    T_.dve(lambda e, hp=hp, dec=dec, ups=ups:
                           e.scalar_tensor_tensor(out=Cst[:, hp, :], in0=ups[:, 0:129], scalar=dec, in1=Cd[:, :], op0=ALU.mult, op1=ALU.add),
                           reads=[("ps", 3), "Cd", ("bexp", bi)], writes=[("C", hp)])
                    T_.act(lambda e, hp=hp: e.activation(out=Cbf[:, hp, :], in_=Cst[:, hp, 0:128], func=AF.Copy),
                           reads=[("C", hp)], writes=[("Cbf", hp)])
                    T_.act(lambda e, hp=hp: e.activation(out=nrep[:, hp, :], in_=Cst[:, hp, 128:129].to_broadcast([128, 128]), func=AF.Copy),
                           reads=[("C", hp)], writes=[("nrep", hp)])

    def _mlstm_views(self, setk):
        if setk == 0:
            EA = self.xin[0][:, 0:1024].rearrange("p (a t) -> p a t", a=8)
            rep = [self.xin[1][:, i * 512:(i + 1) * 512].rearrange("p (h d) -> p h d", h=8) for i in range(2)]
            return EA, rep, self.qt_all, self.kt_all, self.Ka_all, self.mPT
        yt = self.yt
        EA = yt[:, 0:2, :].rearrange("p a (b t) -> p (a b) t", t=128)
        rep = [yt[:, 2 + i, :].rearrange("p (h d) -> p h d", h=8) for i in range(2)]
        qt = yt[:, 4, 0:256].bitcast(BF16).rearrange("p (a t) -> p a t", a=4)
        kt = yt[:, 4, 256:512].bitcast(BF16).rearrange("p (a t) -> p a t", a=4)
        Ka = yt[:, 5, 0:256].bitcast(BF16).rearrange("p (a t) -> p a t", a=4)
        mPT = yt[:, 6, :].bitcast(BF16).rearrange("p (a t) -> p a t", a=8)
        return EA, rep, qt, kt, Ka, mPT

    def _mlstm_set_tokens(self, setk):
        return ([("EA", setk, 0), ("EA", setk, 1), ("rep", setk, 0), ("rep", setk, 1), ("qt_all", setk), ("kt_all", setk),
                 ("Ka_all", setk), ("mPT", setk, 0), ("mPT", setk, 1)])

    def _mlstm_front(self, j, cn, c0, setk):
        T_ = self.T
        qk, Vext, hsg, sgo = self.qk, self.Vext, self.hsg, self.sgo
        ident, identb, maskT, ones = self.ident, self.identb, self.maskT, self.ones_bf
        Cst, Cbf, nrep = self.Cst, self.Cbf, self.nrep
        gsb, lneg, nla = self.gsb, self.lneg, self.nla
        EA, rep, qt_all, kt_all, Ka_all, mPT = self._mlstm_views(setk)
        dns = [self.tmp[0][:, :].rearrange("p (h t) -> p h t", h=4), self.rstd[0][:, :].rearrange("p (h t) -> p h t", h=4)]
        t1s = [self.tmp[1][:, :].rearrange("p (h t) -> p h t", h=4), self.rstd[1][:, :].rearrange("p (h t) -> p h t", h=4)]
        dtoks = [("tmp", 0), ("rstd", 0)]
        ttoks = [("tmp", 1), ("rstd", 1)]
        hbanks = [(6, 7), (0, 1)]
        ps = self.psum
        T_.dve(lambda e: e.tensor_copy(rep[0][0:cn, :, :], lneg[0:cn, j, :].unsqueeze(2).to_broadcast([cn, 8, 64])),
               reads=[("lneg", j)], writes=[("rep", setk, 0)])
        T_.dve(lambda e: e.tensor_copy(rep[1][0:cn, :, :], nla[0:cn, j, :].unsqueeze(2).to_broadcast([cn, 8, 64])),
               reads=[("nla", j)], writes=[("rep", setk, 1)])
        for hp in range(4):
            bank, o = hp // 2, (hp % 2) * 256
            T_.pe(lambda e, hp=hp, bank=bank, o=o:
                  e.matmul(ps[bank][:, o:o + cn], lhsT=rep[0][0:cn, 2 * hp:2 * hp + 2, :].rearrange("p a b -> p (a b)"),
                           rhs=maskT[0:cn, 0:cn], start=True, stop=True),
                  reads=[("rep", setk, 0), "maskT"], writes=[("ps", bank)])
            T_.pe(lambda e, hp=hp, bank=bank, o=o:
                  e.matmul(ps[bank][:, o + 128:o + 128 + cn], lhsT=rep[1][0:cn, 2 * hp:2 * hp + 2, :].rearrange("p a b -> p (a b)"),
                           rhs=ident[0:cn, 0:cn], start=True, stop=True),
                  reads=[("rep", setk, 1), "ident"], writes=[("ps", bank)])
        for b in range(2):
            T_.act(lambda e, b=b: e.activation(out=EA[:, 4 * b:4 * b + 4, 0:cn],
                                               in_=ps[b][:, :].rearrange("p (a t) -> p a t", a=4)[:, :, 0:cn], func=AF.Exp, scale=-1.0),
                   reads=[("ps", b)], writes=[("EA", setk, b)])
        T_.dve(lambda e: e.scalar_tensor_tensor(out=qt_all[:, :, 0:cn], in0=qk[:, 0:4, c0:c0 + cn], scalar=0.125,
                                                in1=EA[:, 0:8:2, 0:cn], op0=ALU.mult, op1=ALU.mult),
               reads=[("qk", c) for c in range(4)] + [("EA", setk, 0), ("EA", setk, 1)], writes=[("qt_all", setk)])
        T_.dve(lambda e: e.tensor_tensor(out=kt_all[:, :, 0:cn], in0=qk[:, 4:8, c0:c0 + cn], in1=EA[:, 1:8:2, 0:cn], op=ALU.mult),
               reads=[("qk", c) for c in range(4, 8)] + [("EA", setk, 0), ("EA", setk, 1)], writes=[("kt_all", setk)])
        for hp in range(4):
            T_.pe(lambda e, hp=hp: e.transpose(self.psum2_bf[0:cn, hp * 128:(hp + 1) * 128], kt_all[:, hp, 0:cn], identb[:]),
                  reads=[("kt_all", setk), "identb"], writes=[("ps", 2)])
        T_.dve(lambda e: e.tensor_copy(Ka_all[0:cn, :, :], self.psum2_bf[0:cn, 0:512].rearrange("p (a d) -> p a d", a=4)),
               reads=[("ps", 2)], writes=[("Ka_all", setk)])
        for hh in range(8):
            hp, r0 = hh // 2, 64 * (hh % 2)
            bank, off = 4 + hh % 2, hp * 128
            T_.pe(lambda e, hp=hp, r0=r0, bank=bank, off=off:
                  e.matmul(ps[bank][0:cn, off:off + cn], lhsT=kt_all[r0:r0 + 64, hp, 0:cn], rhs=qt_all[r0:r0 + 64, hp, 0:cn],
                           start=True, stop=True),
                  reads=[("kt_all", setk), ("qt_all", setk)], writes=[("ps", bank)])
        for a in range(2):
            T_.dve(lambda e, a=a: e.tensor_tensor(out=mPT[0:cn, 4 * a:4 * a + 4, 0:cn],
                                                  in0=ps[4 + a][0:cn, :].rearrange("p (h t) -> p h t", h=4)[:, :, 0:cn],
                                                  in1=maskT[0:cn, 0:cn].unsqueeze(1).to_broadcast([cn, 4, cn]), op=ALU.mult),
                   reads=[("ps", 4 + a), "maskT"], writes=[("mPT", setk, a)])

    def _mlstm_back(self, j, cn, c0, setk):
        T_ = self.T
        qk, Vext, hsg, sgo = self.qk, self.Vext, self.hsg, self.sgo
        ident, identb, maskT, ones = self.ident, self.identb, self.maskT, self.ones_bf
        Cst, Cbf, nrep = self.Cst, self.Cbf, self.nrep
        gsb, lneg, nla = self.gsb, self.lneg, self.nla
        EA, rep, qt_all, kt_all, Ka_all, mPT = self._mlstm_views(setk)
        dns = [self.tmp[0][:, :].rearrange("p (h t) -> p h t", h=4), self.rstd[0][:, :].rearrange("p (h t) -> p h t", h=4)]
        t1s = [self.tmp[1][:, :].rearrange("p (h t) -> p h t", h=4), self.rstd[1][:, :].rearrange("p (h t) -> p h t", h=4)]
        dtoks = [("tmp", 0), ("rstd", 0)]
        ttoks = [("tmp", 1), ("rstd", 1)]
        hbanks = [(6, 7), (0, 1)]
        ps = self.psum
        for a in range(2):
            hb, db = hbanks[a]
            dn, t1, dtok, ttok = dns[a], t1s[a], dtoks[a], ttoks[a]
            for hi in range(4):
                hh = 2 * hi + a
                hp, r0, off = hi, 64 * a, hi * 128
                T_.pe(lambda e, hh=hh, off=off, hi=hi, a=a, hb=hb, db=db: e.matmul(ps[hb][:, off:off + cn], lhsT=Vext[0:cn, j, hh, 0:128], rhs=mPT[0:cn, 4 * a + hi, 0:cn],
                                                         start=True, stop=False),
                      reads=[("V", j, hh // 4), ("mPT", setk, a)], writes=[("ps", hb)])
                T_.pe(lambda e, hp=hp, r0=r0, off=off, hb=hb, db=db: e.matmul(ps[hb][:, off:off + cn], lhsT=Cbf[r0:r0 + 64, hp, :], rhs=qt_all[r0:r0 + 64, hp, 0:cn],
                                                                start=False, stop=True),
                      reads=["Cbfall", ("qt_all", setk)], writes=[("ps", hb)])
                T_.pe(lambda e, hh=hh, off=off, hi=hi, a=a, hb=hb, db=db: e.matmul(ps[db][:, off:off + cn], lhsT=ones[0:cn, :], rhs=mPT[0:cn, 4 * a + hi, 0:cn],
                                                         start=True, stop=False),
                      reads=["ones", ("mPT", setk, a)], writes=[("ps", db)])
                T_.pe(lambda e, hp=hp, r0=r0, off=off, hb=hb, db=db: e.matmul(ps[db][:, off:off + cn], lhsT=nrep[r0:r0 + 64, hp, :], rhs=qt_all[r0:r0 + 64, hp, 0:cn],
                                                                start=False, stop=True),
                      reads=["nrepall", ("qt_all", setk)], writes=[("ps", db)])
            T_.act(lambda e, dn=dn, db=db: e.activation(out=dn[:, :, 0:cn], in_=ps[db][:, :].rearrange("p (h t) -> p h t", h=4)[:, :, 0:cn], func=AF.Abs),
                   reads=[("ps", db)], writes=[dtok])
            T_.dve(lambda e, dn=dn: e.tensor_scalar(dn[:, :, 0:cn], dn[:, :, 0:cn], 1.0, None, ALU.max), reads=[dtok], writes=[dtok])
            T_.act(lambda e, dn=dn: e.activation(out=dn[:, :, 0:cn], in_=dn[:, :, 0:cn], func=AF.Ln), reads=[dtok], writes=[dtok])
            T_.act(lambda e, dn=dn: e.activation(out=dn[:, :, 0:cn], in_=dn[:, :, 0:cn], func=AF.Exp, scale=-1.0), reads=[dtok], writes=[dtok])
            T_.dve(lambda e, dn=dn, t1=t1, hb=hb: e.tensor_tensor(out=t1[:, :, 0:cn], in0=ps[hb][:, :].rearrange("p (h t) -> p h t", h=4)[:, :, 0:cn],
                                             in1=dn[:, :, 0:cn], op=ALU.mult),
                   reads=[("ps", hb), dtok], writes=[ttok])
            T_.dve(lambda e, a=a, t1=t1: e.tensor_tensor(out=hsg[:, a:8:2, c0:c0 + cn], in0=t1[:, :, 0:cn],
                                                  in1=sgo[:, a:8:2, c0:c0 + cn], op=ALU.mult),
                   reads=[ttok] + [("sgo", c) for c in range(a, 8, 2)],
                   writes=[("hsg", c) for c in range(a, 8, 2)])
        for hh in range(8):
            hp, r0 = hh // 2, 64 * (hh % 2)
            bank, coff = 4 + hp // 2, (hp % 2) * 256
            T_.pe(lambda e, hh=hh, hp=hp, r0=r0, bank=bank, coff=coff:
                  e.matmul(ps[bank][r0:r0 + 64, coff:coff + 129], lhsT=Ka_all[0:cn, hp, r0:r0 + 64], rhs=Vext[0:cn, j, hh, 0:129],
                           start=True, stop=True),
                  reads=[("Ka_all", setk), ("V", j, hh // 4)], writes=[("ps", bank)])
        for b in range(2):
            T_.dve(lambda e, b=b: e.tensor_tensor(out=Cst[:, 2 * b:2 * b + 2, :], in0=ps[4 + b][:, 0:512].rearrange("p (a v) -> p a v", a=2)[:, :, 0:129],
                                                  in1=Cst[:, 2 * b:2 * b + 2, :], op=ALU.add),
                   reads=[("ps", 4 + b), "Call"], writes=["Call"])
        T_.dve(lambda e: e.tensor_tensor(out=Cst[:, :, :], in0=Cst[:, :, :], in1=EA[:, 0:8:2, cn - 1:cn].to_broadcast([128, 4, 129]), op=ALU.mult),
               reads=["Call", ("EA", setk, 0), ("EA", setk, 1)], writes=["Call"])
        T_.act(lambda e: e.activation(out=Cbf[:, :, :], in_=Cst[:, :, 0:128], func=AF.Copy), reads=["Call"], writes=["Cbfall"])
        T_.act(lambda e: e.activation(out=nrep[:, :, :], in_=Cst[:, :, 128:129].to_broadcast([128, 4, 128]), func=AF.Copy),
               reads=["Call"], writes=["nrepall"])

    def _mlstm_outproj(self, ti, n):
        T_ = self.T
        w_out = self.a_w_out
        sq, yt, hsg = self.sq, self.yt, self.hsg
        if True:
            for dh in range(2):
                slab, stok = self.wload(w_out[:, dh * 512:(dh + 1) * 512], 8, 512)
                for di in range(4):
                    d = dh * 4 + di
                    yb = di
                    yps = self.psum[yb]
                    for k in range(8):
                        T_.pe(lambda e, k=k, di=di, slab=slab, yps=yps:
                              e.matmul(yps[:, 0:n], lhsT=slab[:, k, di * 128:(di + 1) * 128], rhs=hsg[:, k, 0:n],
                                       start=(k == 0), stop=(k == 7)),
                              reads=[stok, ("hsg", k)], writes=[("ps", yb)])
                    T_.act(lambda e, d=d, yps=yps: e.activation(out=sq[:, d, 0:n], in_=yps[:, 0:n], func=AF.Square),
                           reads=[("ps", yb)], writes=[("sq", d)])
                    T_.act(lambda e, d=d, yps=yps: e.activation(out=yt[:, d, 0:n], in_=yps[:, 0:n], func=AF.Copy,
                                                                scale=self.gain(0, 3, d)),
                           reads=[("ps", yb), "pv"], writes=[("yt", d)])
            self.postnorm_residual2(ti, psb=5, first=True)

    def hgrn2_stage(self):
        T_ = self.T
        pv, lbv, oml = self.pv, self.lbv, self.oml
        self.arena_handoff([("Vb", jb) for jb in range(4)] + [("hsgb", c) for c in range(8)] + [("X", i) for i in range(10)])
        self.arena2_handoff(["Sall", "Sbfall", ("PTa", 0, 0), ("PTa", 0, 1), ("kTa", 0)] + [("decs", c) for c in range(8)])
        x0 = self.xin[0]
        self.PTa_s = [self.PTa, x0[:, 0:512].bitcast(BF16).rearrange("p (h t) -> p h t", h=8)]
        self.kTa_s = [self.kTa, x0[:, 512:1024].bitcast(BF16).rearrange("p (h t) -> p h t", h=8)]
        T_.alias([("PTa", 1, 0), ("PTa", 1, 1), ("kTa", 1)], [("xin", 0)])
        T_.dve(lambda e: e.tensor_tensor(out=lbv[:], in0=pv[:, 152:160], in1=pv[:, 144:152], op=ALU.subtract),
               reads=["pv"], writes=["lbv"])
        T_.act(lambda e: e.activation(out=lbv[:], in_=lbv[:], func=AF.Sigmoid), reads=["lbv"], writes=["lbv"])
        T_.dve(lambda e: e.tensor_scalar(oml[:], lbv[:], -1.0, 1.0, ALU.mult, ALU.add), reads=["lbv"], writes=["oml"])
        T_.dve(lambda e: e.memset(self.Sst[:], 0.0), writes=["Sall"])
        T_.dve(lambda e: e.memset(self.Sbf[:], 0.0), writes=["Sbfall"])
        rot = {"pp": 0, "pv": 0, "ql": 0, "kl": 0, "kT": 0, "dec": 0, "PT": 0}

        def nxt(k, m=2):
            v = rot[k]
            rot[k] = (v + 1) % m
            return v

        for ti, (t0, n) in enumerate(self.TILES):
            self._hg_tile(ti, t0, n, nxt)
        T_.alias([("xin", 0)], [("PTa", 1, 0), ("PTa", 1, 1), ("kTa", 1)])

    def _hg_tile(self, ti, t0, n, nxt):
        T_ = self.T
        w_in = self.b_w_in
        Vb, sgo = self.Vb, self.sgo
        ub, ui = self.prenorm(1, 2, ti, psb=5)
        utoks = [("u", ui, k) for k in range(8)]
        nblk = (n + 127) // 128
        def vg_gen():
            for half in range(2):
                slab, stok = self.wload(w_in[:, 2048 + half * 512: 2048 + (half + 1) * 512], 8, 512, slot=(0, 3)[half])
                for jb in range(nblk):
                    bn = min(128, n - jb * 128)
                    pb = 4 + (jb % 2)
                    pst = self.psum[pb]
                    for k in range(8):
                        T_.pe(lambda e, k=k, jb=jb, bn=bn, slab=slab, pst=pst:
                              e.matmul(pst[0:bn, 0:512], lhsT=ub[:, k, jb * 128:jb * 128 + bn], rhs=slab[:, k, 0:512],
                                       start=(k == 0), stop=(k == 7)),
                              reads=[stok, utoks[k]], writes=[("ps", pb)])
                    T_.act(lambda e, jb=jb, bn=bn, half=half, pst=pst:
                           e.activation(out=Vb[0:bn, jb, half * 512:(half + 1) * 512], in_=pst[0:bn, :], func=AF.Copy),
                           reads=[("ps", pb)], writes=[("Vb", jb)])
                    yield
            for half in range(2):
                slab, stok = self.wload(w_in[:, 3072 + half * 512: 3072 + (half + 1) * 512], 8, 512, slot=(0, 3)[half])
                for cc in range(4):
                    c8 = half * 4 + cc
                    pb = 6 + (cc % 2)
                    pst = self.psum[pb]
                    for k in range(8):
                        T_.pe(lambda e, k=k, cc=cc, slab=slab, pst=pst:
                              e.matmul(pst[:, 0:n], lhsT=slab[:, k, cc * 128:(cc + 1) * 128], rhs=ub[:, k, 0:n],
                                       start=(k == 0), stop=(k == 7)),
                              reads=[stok, utoks[k]], writes=[("ps", pb)])
                    T_.act(lambda e, pst=pst, c8=c8: e.activation(out=sgo[:, c8, 0:n], in_=pst[:, 0:n], func=AF.Sigmoid),
                           reads=[("ps", pb)], writes=[("sgo", c8)])
                    yield
        slabs = {}

        def get_slabs(hg):
            if hg not in slabs:
                q_ = self.wload(w_in[:, hg * 512:(hg + 1) * 512], 8, 512, slot=1)
                f_ = self.wload(w_in[:, 1024 + hg * 512: 1024 + (hg + 1) * 512], 8, 512, slot=2)
                slabs[hg] = (q_, f_)
            return slabs[hg]

        kui = (1 - ui) if ui in (0, 1) else 0
        shared = {"QT": self.sq, "KT": self.ubuf[kui], "kui": kui}

        def front(c):
            (qslab, qtok), (fslab, ftok) = get_slabs(c // 4)
            return self._hg_front(n, ub, utoks, c, c % 4, qslab, qtok, fslab, ftok, nxt, c % 2, shared)

        def drive(gens):
            gens = [g for g in gens if g is not None]
            while gens:
                for g in list(gens):
                    try:
                        next(g)
                    except StopIteration:
                        gens.remove(g)

        vg = vg_gen()
        for c in range(0, 8, 2):
            drive([front(c), front(c + 1)] + ([vg] if c < 4 else []))
        drive([vg])
        self.wslot_next = 0
        self._hg_block_front(0, min(128, n), kui, 0)
        for jb in range(nblk):
            if jb + 1 < nblk:
                self._hg_block_front(jb + 1, min(128, n - (jb + 1) * 128), kui, (jb + 1) % 2)
            self._hg_block_b(jb, min(128, n - jb * 128), kui, jb % 2)
        self._hg_out(ti, n)

    def _hg_front(self, n, ub, utoks, c, cc, qslab, qtok, fslab, ftok, nxt, setk, res):
        T_ = self.T
        pv = self.pv
        X0, X1, X2, X3, X4 = self.X[5 * setk:5 * setk + 5]
        xo = 5 * setk
        qb, fb = 2 * setk, 2 * setk + 1
        qps, fps = self.psum[qb], self.psum[fb]
        for k in range(8):
            T_.pe(lambda e, k=k: e.matmul(qps[:, 0:n], lhsT=qslab[:, k, cc * 128:(cc + 1) * 128], rhs=ub[:, k, 0:n],
                                          start=(k == 0), stop=(k == 7)),
                  reads=[qtok, utoks[k]], writes=[("ps", qb)])
        yield
        T_.act(lambda e: e.activation(out=X0[:, 0:n], in_=qps[:, 0:n], func=AF.Silu), reads=[("ps", qb)], writes=[("X", xo + 0)])
        yield
        for k in range(8):
            T_.pe(lambda e, k=k: e.matmul(fps[:, 0:n], lhsT=fslab[:, k, cc * 128:(cc + 1) * 128], rhs=ub[:, k, 0:n],
                                          start=(k == 0), stop=(k == 7)),
                  reads=[ftok, utoks[k]], writes=[("ps", fb)])
        yield
        T_.act(lambda e: e.activation(out=X1[:, 0:n], in_=fps[:, 0:n], func=AF.Sigmoid, bias=pv[:, 136 + c:137 + c]),
               reads=[("ps", fb), "pv"], writes=[("X", xo + 1)])
        yield
        T_.dve(lambda e: e.tensor_scalar(X1[:, 0:n], X1[:, 0:n], self.oml[:, c:c + 1], self.lbv[:, c:c + 1], ALU.mult, ALU.add),
               reads=[("X", xo + 1), "oml", "lbv"], writes=[("X", xo + 1)])
        yield
        T_.act(lambda e: e.activation(out=X2[:, 0:n], in_=X1[:, 0:n], func=AF.Ln), reads=[("X", xo + 1)], writes=[("X", xo + 2)])
        yield
        T_.dve(lambda e: e.tensor_scalar(X1[:, 0:n], X1[:, 0:n], -1.0, 1.0, ALU.mult, ALU.add),
               reads=[("X", xo + 1), ("X", xo + 2)], writes=[("X", xo + 1)])
        yield
        nch = (n + 63) // 64
        for jc in range(nch):
            if jc == 4:
                yield
            c0 = jc * 64
            cl = min(64, n - c0)
            T_.dve(lambda e, c0=c0, cl=cl: e.tensor_tensor_scan(out=X3[:, c0:c0 + cl], data0=self.ones_f[:, 0:cl],
                                                                 data1=X2[:, c0:c0 + cl], initial=0.0, op0=ALU.mult, op1=ALU.add),
                   reads=[("X", xo + 2), "ones_f"], writes=[("X", xo + 3)])
        yield
        T_.act(lambda e: e.activation(out=X4[:, 0:n], in_=X3[:, 0:n], func=AF.Exp), reads=[("X", xo + 3)], writes=[("X", xo + 4)])
        T_.act(lambda e: e.activation(out=X2[:, 0:n], in_=X3[:, 0:n], func=AF.Exp, scale=-1.0), reads=[("X", xo + 3)], writes=[("X", xo + 2)])
        yield
        QT, KT, kui = res["QT"], res["KT"], res["kui"]
        decs = self.decs_all
        T_.dve(lambda e: e.tensor_tensor(out=QT[:, c, 0:n], in0=X0[:, 0:n], in1=X4[:, 0:n], op=ALU.mult),
               reads=[("X", xo + 0), ("X", xo + 4)], writes=[("sq", c)])
        T_.dve(lambda e: e.tensor_tensor(out=KT[:, c, 0:n], in0=X1[:, 0:n], in1=X2[:, 0:n], op=ALU.mult),
               reads=[("X", xo + 1), ("X", xo + 2)], writes=[("u", kui, c)])
        if n % 64 == 0:
            T_.act(lambda e: e.activation(out=decs[:, c, 0:nch], in_=X4[:, 63:n:64], func=AF.Copy), reads=[("X", xo + 4)], writes=[("decs", c)])
        else:
            assert nch == 1
            T_.act(lambda e: e.activation(out=decs[:, c, 0:1], in_=X4[:, n - 1:n], func=AF.Copy), reads=[("X", xo + 4)], writes=[("decs", c)])
        yield

    def _hg_blocks(self, n, c, r, nxt):
        nblk = (n + 127) // 128
        for jb in range(nblk):
            yield from self._hg_block(c, jb, min(128, n - jb * 128), r["qtl"], r["qi"], r["ktl"], r["ki"], r["decs"], r["di"], nxt)


    def _hg_block(self, c, jb, bn, qtl, qi, ktl, ki, decs, di, nxt):
        T_ = self.T
        Vb, sgo, yt = self.Vb, self.sgo, self.yt
        Sst, Sbf, Sd = self.Sst, self.Sbf, self.Sd
        b0 = jb * 128
        aps, ops, sups = self.psum[4], self.psum[5], self.psum[7]
        T_.pe(lambda e: e.matmul(aps[0:bn, 0:bn], lhsT=ktl[:, b0:b0 + bn], rhs=qtl[:, b0:b0 + bn], start=True, stop=True),
              reads=[("ktl", ki), ("qtl", qi)], writes=[("ps", 4)])
        yield
        pi = nxt("PT")
        PT = self.PT[pi]
        T_.dve(lambda e: e.tensor_tensor(out=PT[0:bn, 0:bn], in0=aps[0:bn, 0:bn], in1=self.maskBD[0:bn, 0:bn], op=ALU.mult),
               reads=[("ps", 4), "maskBD"], writes=[("PT", pi)])
        yield
        T_.pe(lambda e: e.matmul(ops[:, 0:bn], lhsT=Vb[0:bn, jb, c * 128:(c + 1) * 128], rhs=PT[0:bn, 0:bn], start=True, stop=False),
              reads=[("Vb", jb), ("PT", pi)], writes=[("ps", 5)])
        kps = self.psum6_bf[:, 0:128]
        T_.pe(lambda e: e.transpose(kps[0:bn, :], ktl[:, b0:b0 + bn], self.identb[:]),
              reads=[("ktl", ki), "identb"], writes=[("ps", 6)])
        yield
        kti = nxt("kT")
        kTs = self.kTs[kti]
        T_.act(lambda e: e.activation(out=kTs[0:bn, :], in_=kps[0:bn, :], func=AF.Copy), reads=[("ps", 6)], writes=[("kTs", kti)])
        chunks = [(0, min(64, bn))] + ([(64, bn - 64)] if bn > 64 else [])
        for e_i, (r0, cl) in enumerate(chunks):
            yield
            yield from self._hg_chunk(c, jb, b0, r0, cl, e_i == len(chunks) - 1, qtl, qi, kTs, kti, decs, di, 2 * jb + e_i)
        yield
        T_.dve(lambda e: e.tensor_tensor(out=yt[:, c, b0:b0 + bn], in0=ops[:, 0:bn], in1=sgo[:, c, b0:b0 + bn], op=ALU.mult),
               reads=[("ps", 5), ("sgo", c)], writes=[("yt", c)])

    def _hg_chunk(self, c, jb, b0, r0, cl, last, qtl, qi, kTs, kti, decs, di, jc):
        T_ = self.T
        Vb = self.Vb
        Sst, Sbf, Sd = self.Sst, self.Sbf, self.Sd
        ops, sups = self.psum[5], self.psum[7]
        T_.pe(lambda e: e.matmul(ops[:, r0:r0 + cl], lhsT=Sbf[:, c, :], rhs=qtl[:, b0 + r0:b0 + r0 + cl], start=False, stop=last),
              reads=[("Sbf", c), ("qtl", qi)], writes=[("ps", 5)])
        T_.pe(lambda e: e.matmul(sups[:, 0:128], lhsT=kTs[r0:r0 + cl, :], rhs=Vb[r0:r0 + cl, jb, c * 128:(c + 1) * 128], start=True, stop=True),
              reads=[("kTs", kti), ("Vb", jb)], writes=[("ps", 7)])
        yield
        dec = decs[:, jc:jc + 1]
        T_.dve(lambda e: e.tensor_scalar(Sd[:, :], Sst[:, c, :], dec, None, ALU.mult),
               reads=[("S", c), ("decs", di)], writes=["Sd"])
        T_.dve(lambda e: e.scalar_tensor_tensor(out=Sst[:, c, :], in0=sups[:, 0:128], scalar=dec, in1=Sd[:, :], op0=ALU.mult, op1=ALU.add),
               reads=[("ps", 7), "Sd", ("decs", di)], writes=[("S", c)])
        yield
        T_.act(lambda e: e.activation(out=Sbf[:, c, :], in_=Sst[:, c, :], func=AF.Copy), reads=[("S", c)], writes=[("Sbf", c)])

    def _hg_block_front(self, jb, bn, kui, setk):
        T_ = self.T
        QT, KT = self.sq, self.ubuf[kui]
        Vb, sgo, yt = self.Vb, self.sgo, self.yt
        PTa, kTa = self.PTa_s[setk], self.kTa_s[setk]
        b0 = jb * 128
        for hh in range(8):
            bank, off = 4 + hh // 4, (hh % 4) * 128
            T_.pe(lambda e, hh=hh, bank=bank, off=off:
                  e.matmul(self.psum[bank][0:bn, off:off + bn], lhsT=KT[:, hh, b0:b0 + bn], rhs=QT[:, hh, b0:b0 + bn],
                           start=True, stop=True),
                  reads=[("u", kui, hh), ("sq", hh)], writes=[("ps", bank)])
        for a in range(2):
            T_.dve(lambda e, a=a:
                   e.tensor_tensor(out=PTa[0:bn, 4 * a:4 * a + 4, 0:bn],
                                   in0=self.psum[4 + a][0:bn, :].rearrange("p (h t) -> p h t", h=4)[:, :, 0:bn],
                                   in1=self.maskBD[0:bn, 0:bn].unsqueeze(1).to_broadcast([bn, 4, bn]), op=ALU.mult),
                   reads=[("ps", 4 + a), "maskBD"], writes=[("PTa", setk, a)])
        for hh in range(8):
            T_.pe(lambda e, hh=hh: e.transpose(self.psum6_bf[0:bn, hh * 128:(hh + 1) * 128], KT[:, hh, b0:b0 + bn], self.identb[:]),
                  reads=[("u", kui, hh), "identb"], writes=[("ps", 6)])
        T_.act(lambda e: e.activation(out=kTa[0:bn, :, :], in_=self.psum6_bf[0:bn, 0:1024].rearrange("p (h d) -> p h d", h=8), func=AF.Copy),
               reads=[("ps", 6)], writes=[("kTa", setk)])

    def _hg_block_b(self, jb, bn, kui, setk):
        T_ = self.T
        QT, KT = self.sq, self.ubuf[kui]
        Vb, sgo, yt = self.Vb, self.sgo, self.yt
        Sst, Sbf, decs = self.Sst, self.Sbf, self.decs_all
        PTa, kTa = self.PTa_s[setk], self.kTa_s[setk]
        b0 = jb * 128
        chunks = [(0, min(64, bn))] + ([(64, bn - 64)] if bn > 64 else [])
        for e_i, (r0, cl) in enumerate(chunks):
            jc = 2 * jb + e_i
            for hh in range(8):
                bank, off = hh // 4, (hh % 4) * 128
                T_.pe(lambda e, hh=hh, bank=bank, off=off, r0=r0, cl=cl:
                      e.matmul(self.psum[bank][:, off + r0:off + r0 + cl], lhsT=Vb[r0:r0 + cl, jb, hh * 128:(hh + 1) * 128],
                               rhs=PTa[r0:r0 + cl, hh, r0:r0 + cl], start=True, stop=False),
                      reads=[("Vb", jb), ("PTa", setk, hh // 4)], writes=[("ps", bank)])
                T_.pe(lambda e, hh=hh, bank=bank, off=off, r0=r0, cl=cl:
                      e.matmul(self.psum[bank][:, off + r0:off + r0 + cl], lhsT=Sbf[:, hh, :],
                               rhs=QT[:, hh, b0 + r0:b0 + r0 + cl], start=False, stop=True),
                      reads=["Sbfall", ("sq", hh)], writes=[("ps", bank)])
            for hh in range(8):
                bank, off = 2 + hh // 4, (hh % 4) * 128
                T_.pe(lambda e, hh=hh, bank=bank, off=off, r0=r0, cl=cl:
                      e.matmul(self.psum[bank][:, off:off + 128], lhsT=kTa[r0:r0 + cl, hh, :],
                               rhs=Vb[r0:r0 + cl, jb, hh * 128:(hh + 1) * 128], start=True, stop=True),
                      reads=[("kTa", setk), ("Vb", jb)], writes=[("ps", bank)])
            for a in range(2):
                T_.dve(lambda e, a=a:
                       e.tensor_tensor(out=Sst[:, 4 * a:4 * a + 4, :], in0=self.psum[2 + a][:, :].rearrange("p (h v) -> p h v", h=4),
                                       in1=Sst[:, 4 * a:4 * a + 4, :], op=ALU.add),
                       reads=[("ps", 2 + a), "Sall"], writes=["Sall"])
            T_.dve(lambda e, jc=jc:
                   e.tensor_tensor(out=Sst[:, :, :], in0=Sst[:, :, :],
                                   in1=decs[:, :, jc:jc + 1].to_broadcast([128, 8, 128]), op=ALU.mult),
                   reads=["Sall"] + [("decs", c) for c in range(8)], writes=["Sall"])
            T_.act(lambda e: e.activation(out=Sbf[:, :, :], in_=Sst[:, :, :], func=AF.Copy), reads=["Sall"], writes=["Sbfall"])
        for a in range(2):
            T_.dve(lambda e, a=a:
                   e.tensor_tensor(out=yt[:, 4 * a:4 * a + 4, b0:b0 + bn],
                                   in0=self.psum[a][:, :].rearrange("p (h t) -> p h t", h=4)[:, :, 0:bn],
                                   in1=sgo[:, 4 * a:4 * a + 4, b0:b0 + bn], op=ALU.mult),
                   reads=[("ps", a)] + [("sgo", c) for c in range(4 * a, 4 * a + 4)],
                   writes=[("yt", c) for c in range(4 * a, 4 * a + 4)])

    def _hg_out(self, ti, n):
        T_ = self.T
        w_out = self.b_w_out
        sq, yt, hsgb, pv = self.sq, self.yt, self.hsgb, self.pv
        for c in range(NCH):
            T_.act(lambda e, c=c: e.activation(out=sq[:, c, 0:n], in_=yt[:, c, 0:n], func=AF.Square),
                   reads=[("yt", c)], writes=[("sq", c)])
        rs, rtok = self.rstd_from_sq([("sq", c) for c in range(NCH)], n, 5)
        for c in range(NCH):
            T_.dve(lambda e, c=c: e.scalar_tensor_tensor(out=hsgb[:, c, 0:n], in0=yt[:, c, 0:n], scalar=pv[:, 160 + c:161 + c],
                                                         in1=rs[:, 0:n], op0=ALU.mult, op1=ALU.mult),
                   reads=[("yt", c), rtok, "pv"], writes=[("hsgb", c), ("X", c // 2)])
        for dh in range(2):
            slab, stok = self.wload(w_out[:, dh * 512:(dh + 1) * 512], 8, 512)
            for di in range(4):
                d = dh * 4 + di
                yb = di
                yps = self.psum[yb]
                for k in range(8):
                    T_.pe(lambda e, k=k, di=di, slab=slab, yps=yps:
                          e.matmul(yps[:, 0:n], lhsT=slab[:, k, di * 128:(di + 1) * 128], rhs=hsgb[:, k, 0:n],
                                   start=(k == 0), stop=(k == 7)),
                          reads=[stok, ("hsgb", k), ("X", k // 2)], writes=[("ps", yb)])
                T_.act(lambda e, d=d, yps=yps: e.activation(out=sq[:, d, 0:n], in_=yps[:, 0:n], func=AF.Square),
                       reads=[("ps", yb)], writes=[("sq", d)])
                T_.act(lambda e, d=d, yps=yps: e.activation(out=yt[:, d, 0:n], in_=yps[:, 0:n], func=AF.Copy,
                                                            scale=self.gain(1, 3, d)),
                       reads=[("ps", yb), "pv"], writes=[("yt", d)])
        self.postnorm_residual2(ti, psb=5, first=True)


def pack_pvec(norm_gains, a_conv_w, a_conv_b, b_f_bias, b_lb_raw, b_g_norm):
    pv = np.zeros((256, 128), np.float32)
    pv[0:96] = np.asarray(norm_gains, np.float32).reshape(2 * 6 * 8, 128)
    pv[96:128] = np.asarray(a_conv_w, np.float32).reshape(4 * 8, 128)
    pv[128:136] = np.asarray(a_conv_b, np.float32).reshape(8, 128)
    pv[136:144] = np.asarray(b_f_bias, np.float32).reshape(8, 128)
    pv[144:160] = np.asarray(b_lb_raw, np.float32).reshape(16, 128)
    pv[160:168] = np.asarray(b_g_norm, np.float32).reshape(8, 128)
    return pv


def make_in_maps(inputs, n_cores, nseq):
    x = np.ascontiguousarray(np.asarray(inputs["x"], np.float32))
    pv = pack_pvec(inputs["norm_gains"], inputs["a_conv_w"], inputs["a_conv_b"], inputs["b_f_bias"],
                   inputs["b_lb_raw"], inputs["b_g_norm"])
    common = {
        "meta": np.ascontiguousarray(np.asarray(inputs["meta_tokens"], np.float32)),
        "pvec": pv,
        "gateb": np.ascontiguousarray(np.asarray(inputs["a_gate_b"], np.float32).reshape(1, 16)),
        "ffn_w_in": np.ascontiguousarray(np.asarray(inputs["ffn_w_in"], np.float32)),
        "ffn_w_out": np.ascontiguousarray(np.asarray(inputs["ffn_w_out"], np.float32)),
        "a_w_in": np.ascontiguousarray(np.asarray(inputs["a_w_in"], np.float32)[0]),
        "a_w_out": np.ascontiguousarray(np.asarray(inputs["a_w_out"], np.float32)[0]),
        "b_w_in": np.ascontiguousarray(np.asarray(inputs["b_w_in"], np.float32)[0]),
        "b_w_out": np.ascontiguousarray(np.asarray(inputs["b_w_out"], np.float32)[0]),
    }
    maps = []
    for i in range(n_cores):
        m = dict(common)
        m["x"] = x[i * nseq:(i + 1) * nseq]
        maps.append(m)
    return maps


def kernel(**inputs):
    n_cores = 8
    nseq = 2
    b = Builder(nseq=nseq)
    nc = b.build()
    in_maps = make_in_maps(inputs, n_cores, nseq)
    res = run_bass_kernel_spmd(nc, in_maps, core_ids=list(range(n_cores)))
    outs = [np.asarray(r["out"]) for r in res.results]
    return np.concatenate(outs, axis=0).astype(np.float32)
```
